# Optimizing a Trainium2 kernel written in Bass

```python
import math
import jax, jax.numpy as jnp
from jax import lax
import numpy as np

D_MODEL = 1024
BATCH = 8
SEQ = 2048
DEPTH = 1

CHUNK = 64
SB_HEADS = 8
SB_HEAD_DIM = 64
SB_WIDTH = SB_HEADS * SB_HEAD_DIM
SB_BLOCK = 128
GLA_HEADS = 4
GLA_KEY_DIM = 64
GLA_VALUE_DIM = 128
GLA_K_WIDTH = GLA_HEADS * GLA_KEY_DIM
GLA_V_WIDTH = GLA_HEADS * GLA_VALUE_DIM
GLA_GATE_RANK = 16
GLA_GATE_TEMP = 16.0
N_BRANCHES = 2
N_EXPERTS = 64
TOP_K = 8
N_GROUPS = 8
TOPK_GROUPS = 4
EXPERT_DIM = 256
SHARED_DIM = 256
ROUTED_SCALE = 2.5
DN_ALPHA = (2 * DEPTH) ** 0.25
DN_BETA = (8 * DEPTH) ** -0.25
LN_EPS = 1e-5
IN_WIDTHS = (SB_WIDTH, SB_WIDTH, SB_WIDTH, GLA_K_WIDTH, GLA_K_WIDTH, GLA_V_WIDTH, GLA_V_WIDTH,
             GLA_GATE_RANK, N_BRANCHES * D_MODEL)
IN_WIDTH = 3 * SB_WIDTH + 2 * GLA_K_WIDTH + 2 * GLA_V_WIDTH + GLA_GATE_RANK + N_BRANCHES * D_MODEL

kernel_name = "hybrid_sb_gla_moe_deepnorm_adaln"


def _split_points():
    pts, acc = [], 0
    for w in IN_WIDTHS[:-1]:
        acc += w
        pts.append(acc)
    return pts


def layer_norm(x, g=None, b=None):
    xf = x.astype(jnp.float32)
    mu = jnp.mean(xf, axis=-1, keepdims=True)
    var = jnp.mean(jnp.square(xf - mu), axis=-1, keepdims=True)
    y = (xf - mu) * lax.rsqrt(var + LN_EPS)
    if g is not None:
        y = y * g.astype(jnp.float32) + b.astype(jnp.float32)
    return y.astype(x.dtype)


def split_heads(t, n_heads):
    b, s, w = t.shape
    return t.reshape(b, s, n_heads, w // n_heads).transpose(0, 2, 1, 3)


def merge_heads(t):
    b, h, s, d = t.shape
    return t.transpose(0, 2, 1, 3).reshape(b, s, h * d)


def stick_breaking_attention(q, k, v):
    q, k, v = (t.astype(jnp.float32) for t in (q, k, v))
    seq = q.shape[2]
    scale = 1.0 / math.sqrt(q.shape[-1])
    outs = []
    for i in range(seq // SB_BLOCK):
        kv_len = (i + 1) * SB_BLOCK
        q_blk = q[:, :, i * SB_BLOCK:kv_len]
        k_blk, v_blk = k[:, :, :kv_len], v[:, :, :kv_len]
        z = jnp.einsum('bhqd,bhkd->bhqk', q_blk, k_blk) * scale
        q_pos = i * SB_BLOCK + jnp.arange(SB_BLOCK)
        k_pos = jnp.arange(kv_len)
        strict = k_pos[None, :] < q_pos[:, None]
        log_beta = jnp.where(strict, jax.nn.log_sigmoid(z), -jnp.inf)
        log_1m = jnp.where(strict, jax.nn.log_sigmoid(-z), 0.0)
        rest = lax.cumsum(log_1m, axis=3, reverse=True) - log_1m
        w = jnp.exp(log_beta + rest)
        outs.append(jnp.einsum('bhqk,bhkd->bhqd', w, v_blk))
    return jnp.concatenate(outs, axis=2)


def gated_linear_attention(q, k, v, log_a):
    q, k, v, log_a = (t.astype(jnp.float32) for t in (q, k, v, log_a))
    b, h, seq, dk = q.shape
    dv = v.shape[-1]
    n_chunks = seq // CHUNK

    def to_chunks(t):
        return jnp.moveaxis(t.reshape(b, h, n_chunks, CHUNK, t.shape[-1]), 2, 0)

    qc, kc, vc = to_chunks(q), to_chunks(k), to_chunks(v)
    gc = jnp.cumsum(to_chunks(log_a), axis=3)
    causal = jnp.tril(jnp.ones((CHUNK, CHUNK), dtype=bool))

    def step(state, inp):
        q_c, k_c, v_c, g_c = inp
        o_inter = jnp.einsum('bhtd,bhdv->bhtv', q_c * jnp.exp(g_c), state)
        diff = g_c[:, :, :, None, :] - g_c[:, :, None, :, :]
        decay = jnp.exp(jnp.where(causal[:, :, None], diff, -jnp.inf))
        scores = jnp.einsum('bhtd,bhsd,bhtsd->bhts', q_c, k_c, decay)
        o_intra = jnp.einsum('bhts,bhsv->bhtv', scores, v_c)
        g_last = g_c[:, :, -1]
        k_dec = k_c * jnp.exp(g_last[:, :, None, :] - g_c)
        state = jnp.exp(g_last)[..., None] * state + jnp.einsum('bhsd,bhsv->bhdv', k_dec, v_c)
        return state, o_inter + o_intra

    s0 = jnp.zeros((b, h, dk, dv), jnp.float32)
    _, o = lax.scan(step, s0, (qc, kc, vc, gc))
    return jnp.moveaxis(o, 0, 2).reshape(b, h, seq, dv)


def token_mixer(h, w_in, gla_w_gate_up, gla_b_gate, gla_norm_w, w_branch_sb, w_branch_gla, w_out):
    proj = h @ w_in
    q_sb, k_sb, v_sb, q_g, k_g, v_g, r_g, g_lr, merge = jnp.split(proj, _split_points(), axis=-1)
    o_sb = merge_heads(stick_breaking_attention(split_heads(q_sb, SB_HEADS), split_heads(k_sb, SB_HEADS),
                                                split_heads(v_sb, SB_HEADS))).astype(h.dtype)
    log_a = jax.nn.log_sigmoid((g_lr @ gla_w_gate_up + gla_b_gate).astype(jnp.float32)) / GLA_GATE_TEMP
    o_g = gated_linear_attention(split_heads(q_g, GLA_HEADS) * (GLA_KEY_DIM ** -0.5),
                                 split_heads(k_g, GLA_HEADS), split_heads(v_g, GLA_HEADS),
                                 split_heads(log_a, GLA_HEADS))
    o_g = o_g * lax.rsqrt(jnp.mean(jnp.square(o_g), axis=-1, keepdims=True) + LN_EPS)
    o_g = (merge_heads(o_g) * gla_norm_w.astype(jnp.float32)).astype(h.dtype) * jax.nn.silu(r_g)
    gate_sb, gate_gla = jnp.split(jax.nn.sigmoid(merge), 2, axis=-1)
    y = gate_sb * (o_sb @ w_branch_sb) + gate_gla * (o_g @ w_branch_gla)
    return y @ w_out


def moe_ffn(h, w_router, router_bias, w_exp_gate_up, w_exp_down, w_shared_gate_up, w_shared_down):
    b, s, d = h.shape
    scores = jax.nn.sigmoid((h @ w_router).astype(jnp.float32))
    sel = scores + router_bias.astype(jnp.float32)
    grp = sel.reshape(b, s, N_GROUPS, N_EXPERTS // N_GROUPS)
    group_score = jnp.sum(lax.top_k(grp, 2)[0], axis=-1)
    _, top_groups = lax.top_k(group_score, TOPK_GROUPS)
    group_mask = jnp.sum(jax.nn.one_hot(top_groups, N_GROUPS, dtype=jnp.float32), axis=-2)
    expert_mask = jnp.repeat(group_mask, N_EXPERTS // N_GROUPS, axis=-1)
    _, idx = lax.top_k(jnp.where(expert_mask > 0, sel, -jnp.inf), TOP_K)
    w = jnp.take_along_axis(scores, idx, axis=-1)
    w = w / jnp.sum(w, axis=-1, keepdims=True) * ROUTED_SCALE
    gates = jnp.sum(jax.nn.one_hot(idx, N_EXPERTS, dtype=jnp.float32) * w[..., None], axis=-2)
    gates = gates.astype(h.dtype)

    def routed_row(args):
        hr, gr = args
        gu = jnp.einsum('td,edf->tef', hr, w_exp_gate_up)
        g_part, u_part = jnp.split(gu, 2, axis=-1)
        act = jax.nn.silu(g_part) * u_part * gr[..., None]
        return jnp.einsum('tef,efd->td', act, w_exp_down)

    routed = lax.map(routed_row, (h, gates))
    sg, su = jnp.split(h @ w_shared_gate_up, 2, axis=-1)
    shared = (jax.nn.silu(sg) * su) @ w_shared_down
    return routed + shared


def setup_inputs(seed: int = 0) -> dict:
    key = jax.random.key(seed)
    ks = jax.random.split(key, 24)

    def nrm(k, shape, scale):
        return jax.random.normal(k, shape, jnp.float32) * scale

    L, D, E = DEPTH, D_MODEL, N_EXPERTS
    return {
        "x": nrm(ks[0], (BATCH, SEQ, D), 1.0),
        "c": nrm(ks[1], (BATCH, D), 1.0),
        "w_ada": nrm(ks[2], (L, D, 6 * D), 0.5 * D ** -0.5),
        "b_ada": nrm(ks[3], (L, 6 * D), 0.02),
        "w_in": nrm(ks[4], (L, D, IN_WIDTH), D ** -0.5),
        "gla_w_gate_up": nrm(ks[5], (L, GLA_GATE_RANK, GLA_K_WIDTH), GLA_GATE_RANK ** -0.5),
        "gla_b_gate": 1.0 + nrm(ks[6], (L, GLA_K_WIDTH), 0.1),
        "gla_norm_w": 1.0 + nrm(ks[7], (L, GLA_V_WIDTH), 0.02),
        "w_branch_sb": nrm(ks[8], (L, SB_WIDTH, D), SB_WIDTH ** -0.5),
        "w_branch_gla": nrm(ks[9], (L, GLA_V_WIDTH, D), GLA_V_WIDTH ** -0.5),
        "w_out": nrm(ks[10], (L, D, D), DN_BETA * D ** -0.5),
        "ln1_g": 1.0 + nrm(ks[11], (L, D), 0.02),
        "ln1_b": nrm(ks[12], (L, D), 0.02),
        "w_router": nrm(ks[13], (L, D, E), D ** -0.5),
        "router_bias": nrm(ks[14], (L, E), 0.01),
        "w_exp_gate_up": nrm(ks[15], (L, E, D, 2 * EXPERT_DIM), D ** -0.5),
        "w_exp_down": nrm(ks[16], (L, E, EXPERT_DIM, D), DN_BETA * EXPERT_DIM ** -0.5),
        "w_shared_gate_up": nrm(ks[17], (L, D, 2 * SHARED_DIM), D ** -0.5),
        "w_shared_down": nrm(ks[18], (L, SHARED_DIM, D), DN_BETA * SHARED_DIM ** -0.5),
        "ln2_g": 1.0 + nrm(ks[19], (L, D), 0.02),
        "ln2_b": nrm(ks[20], (L, D), 0.02),
    }


def reference(x, c, w_ada, b_ada, w_in, gla_w_gate_up, gla_b_gate, gla_norm_w, w_branch_sb, w_branch_gla,
              w_out, ln1_g, ln1_b, w_router, router_bias, w_exp_gate_up, w_exp_down, w_shared_gate_up,
              w_shared_down, ln2_g, ln2_b):
    for l in range(DEPTH):
        mod = jax.nn.silu(c) @ w_ada[l] + b_ada[l]
        shift1, scale1, gate1, shift2, scale2, gate2 = (m[:, None, :] for m in jnp.split(mod, 6, axis=-1))
        h = layer_norm(x) * (1.0 + scale1) + shift1
        mix = token_mixer(h, w_in[l], gla_w_gate_up[l], gla_b_gate[l], gla_norm_w[l],
                          w_branch_sb[l], w_branch_gla[l], w_out[l])
        x = layer_norm(DN_ALPHA * x + gate1 * mix, ln1_g[l], ln1_b[l])
        h = layer_norm(x) * (1.0 + scale2) + shift2
        ffn = moe_ffn(h, w_router[l], router_bias[l], w_exp_gate_up[l], w_exp_down[l],
                      w_shared_gate_up[l], w_shared_down[l])
        x = layer_norm(DN_ALPHA * x + gate2 * ffn, ln2_g[l], ln2_b[l])
    return x
```

```python
import numpy as np
from contextlib import ExitStack
import concourse.bass as bass
import concourse.mybir as mybir
from concourse.bass_utils import run_bass_kernel_spmd

F32 = mybir.dt.float32
BF16 = mybir.dt.bfloat16
F32R = mybir.dt.float32r
AF = mybir.ActivationFunctionType
ALU = mybir.AluOpType
AX = mybir.AxisListType

T = 2048
D = 1024
NT = 16
NG = 4
E = 64
ALPHA = 2.0 ** 0.25
EPS = 1e-5
NEG = -30000.0
ENGS = ("pe", "act", "dve", "pool", "sp")


class _Op:
    __slots__ = ("eng", "fn", "deps", "is_dma", "dma_sem", "token", "signal")


class Sched:
    def __init__(self, nc, stack):
        self.nc = nc
        self.stack = stack
        self.ops = []
        self.last_w = {}
        self.readers = {}
        self.dma_sems = {}
        self.dma_cnt = {}
        self.stopped = False

    def _mk(self, eng, fn, reads, writes):
        op = _Op()
        if self.stopped:
            return op
        op.eng = eng
        op.fn = fn
        op.is_dma = False
        op.dma_sem = None
        op.signal = False
        op.token = None
        deps = set()
        reads = list(reads) + ["PHASE"]
        for r in reads:
            w = self.last_w.get(r)
            if w is not None:
                deps.add(w)
        for r in writes:
            w = self.last_w.get(r)
            if w is not None:
                deps.add(w)
            for rd in self.readers.get(r, ()):
                deps.add(rd)
        op.deps = deps
        i = len(self.ops)
        self.ops.append(op)
        for r in reads:
            if r.startswith("c:"):
                continue
            self.readers.setdefault(r, []).append(i)
        for r in writes:
            self.last_w[r] = i
            self.readers[r] = []
        return op

    def add(self, eng, fn, reads=(), writes=()):
        self._mk(eng, fn, reads, writes)

    def pe(self, fn, reads=(), writes=()):
        self._mk("pe", fn, reads, writes)

    def act(self, fn, reads=(), writes=()):
        self._mk("act", fn, reads, writes)

    def dve(self, fn, reads=(), writes=()):
        self._mk("dve", fn, reads, writes)

    def pool(self, fn, reads=(), writes=()):
        self._mk("pool", fn, reads, writes)

    def dma(self, queue, fn, semkey, reads=(), writes=()):
        op = self._mk(queue, fn, reads, writes)
        if self.stopped:
            return
        op.is_dma = True
        if semkey not in self.dma_sems:
            self.dma_sems[semkey] = self.stack.enter_context(self.nc.semaphore("d_" + semkey))
            self.dma_cnt[semkey] = 0
        self.dma_cnt[semkey] += 16
        op.dma_sem = self.dma_sems[semkey]
        op.token = (semkey, self.dma_cnt[semkey])
        op.signal = True

    def barrier(self, dummy):
        op = self._mk("pool", lambda e: e.memset(dummy, 0.0), [], ["PHASE"])
        return op

    def emit(self, final_keys=()):
        nc = self.nc
        ops = self.ops

        def need_sync(p, op):
            if p.is_dma or op.is_dma:
                return True
            if p.eng != op.eng:
                return True
            return p.eng != "pe"

        for op in ops:
            for d in op.deps:
                p = ops[d]
                if not p.is_dma and need_sync(p, op):
                    p.signal = True
        cnt = {e: 0 for e in ENGS}
        for op in ops:
            if not op.is_dma and op.signal:
                cnt[op.eng] += 1
                op.token = (op.eng, cnt[op.eng])
        eng_sem = {e: self.stack.enter_context(nc.semaphore("e_" + e)) for e in ENGS}
        per_eng = {e: [] for e in ENGS}
        for op in ops:
            per_eng[op.eng].append(op)

        def semof(k):
            return eng_sem[k] if k in eng_sem else self.dma_sems[k]

        def run(name, eng):
            waited = {}
            for op in per_eng[name]:
                need = {}
                for d in op.deps:
                    p = ops[d]
                    if not need_sync(p, op):
                        continue
                    k, v = p.token
                    if need.get(k, 0) < v:
                        need[k] = v
                for k, v in need.items():
                    if waited.get(k, 0) >= v:
                        continue
                    eng.wait_ge(semof(k), v)
                    waited[k] = v
                ins = op.fn(eng)
                if op.is_dma:
                    ins.then_inc(op.dma_sem, 16)
                elif op.signal:
                    ins.then_inc(eng_sem[op.eng], 1)
            if name == "sp":
                for k in final_keys:
                    if k in self.dma_sems:
                        eng.wait_ge(self.dma_sems[k], self.dma_cnt[k])

        with nc.Block() as block:
            @block.tensor
            def _(e):
                run("pe", e)

            @block.scalar
            def _(e):
                run("act", e)

            @block.vector
            def _(e):
                run("dve", e)

            @block.gpsimd
            def _(e):
                run("pool", e)

            @block.sync
            def _(e):
                run("sp", e)


def build(dbg=None, upto=None):
    dbg = dbg or {}
    nc = bass.Bass("TRN2", target_bir_lowering=False)

    def din(name, shape):
        return nc.dram_tensor(name, list(shape), F32, kind="ExternalInput").ap()

    x_d = din("x", [T, D])
    ccol_d = din("c_col", [128, 8])
    wada_d = din("w_ada", [D, 6 * D])
    bada_d = din("b_ada", [1, 6 * D])
    win_d = din("w_in", [D, 5136])
    wup_d = din("gla_w_gate_up", [16, 256])
    bgate_d = din("gla_b_gate", [1, 256])
    normw_d = din("gla_norm_w_col", [128, 4])
    wsb_d = din("w_branch_sb", [512, D])
    wgla_d = din("w_branch_gla", [512, D])
    wout_d = din("w_out", [D, D])
    ln1g_d = din("ln1_g", [1, D])
    ln1b_d = din("ln1_b", [1, D])
    wr_d = din("w_router", [D, E])
    rb_d = din("router_bias", [1, E])
    wgu_d = din("w_exp_gate_up", [E, D, 512])
    wd_d = din("w_exp_down", [E, 256, D])
    wsgu_d = din("w_shared_gate_up", [D, 512])
    wsd_d = din("w_shared_down", [256, D])
    ln2g_d = din("ln2_g", [1, D])
    ln2b_d = din("ln2_b", [1, D])
    out_d = nc.dram_tensor("out", [T, D], F32, kind="ExternalOutput").ap()
    dbg_d = {k: nc.dram_tensor("dbg_" + k, list(shp), F32, kind="ExternalOutput").ap() for k, shp in dbg.items()}

    with ExitStack() as st:
        ARW = 212000 // 4
        AR = st.enter_context(nc.sbuf_tensor("arena", [128, ARW], F32))
        PS = st.enter_context(nc.psum_tensor("psum_all", [128, 8 * 512], F32))[:]
        banks = [PS[:, i * 512:(i + 1) * 512] for i in range(8)]

        def bank2(b0):
            return PS[:, b0 * 512:(b0 + 2) * 512].rearrange("p (b n) -> p b n", b=2)
        S = Sched(nc, st)

        def carve(off, shape, dt):
            n = int(np.prod(shape[1:]))
            nb = n * (4 if dt == F32 else 2)
            assert off % 4 == 0 and nb % 4 == 0, (off, nb)
            assert off + nb <= ARW * 4, ("arena overflow", off, nb)
            a = AR[:, off // 4:(off + nb) // 4]
            if dt != F32:
                a = a.bitcast(dt)
            if len(shape) == 3:
                a = a.rearrange("p (a b) -> p a b", a=shape[1])
            return a

        class Alloc:
            def __init__(self, base, limit):
                self.off = base
                self.limit = limit

            def __call__(self, shape, dt):
                n = int(np.prod(shape[1:])) * (4 if dt == F32 else 2)
                n = (n + 31) // 32 * 32
                a = carve(self.off, shape, dt)
                self.off += n
                assert self.off <= self.limit, ("region overflow", self.off, self.limit)
                return a

        P_BASE, ACC_BASE, HT_BASE, OSB_BASE, OG_BASE, FREE_BASE, END = 0, 18432, 83968, 116736, 133120, 149504, 212000
        pa = Alloc(P_BASE, ACC_BASE)
        ident_bf = pa([128, 128], BF16)
        ident_f = pa([128, 128], F32)
        ones_f = pa([128, 128], F32)
        onesdiv_f = pa([128, 128], F32)
        modcol = pa([128, 32], F32)
        gate1_bc = pa([128, D], F32)
        gate2_bc = pa([128, D], F32)
        lnbc = pa([128, 2, D], F32)
        dummy = pa([128, 8], F32)
        ACC = carve(ACC_BASE, [128, NT, D], F32)
        HT = carve(HT_BASE, [128, 8, T], BF16)
        OSB = carve(OSB_BASE, [128, 4, T], BF16)
        OG = carve(OG_BASE, [128, 4, T], BF16)

        win_v = win_d.rearrange("(kc p) n -> p kc n", p=128)

        def dump(name, ap, reads):
            if name in dbg_d:
                S.dma("sp", lambda e: e.dma_start(out=dbg_d[name], in_=ap), "dbg", reads=reads)

        S.pool(lambda e: e.memset(ident_f, 0.0), writes=["c:identf"])
        S.pool(lambda e: e.affine_select(out=ident_f, in_=ident_f, pattern=[[-1, 128]], compare_op=ALU.not_equal,
                                         fill=1.0, base=0, channel_multiplier=1), reads=["c:identf"], writes=["c:identf"])
        S.pool(lambda e: e.tensor_copy(ident_bf, ident_f), reads=["c:identf"], writes=["c:identbf"])
        S.pool(lambda e: e.memset(ones_f, 1.0), writes=["c:ones"])
        S.pool(lambda e: e.memset(onesdiv_f, 1.0 / 128.0), writes=["c:onesdiv"])

        a0 = Alloc(ACC_BASE, HT_BASE)
        wada_s = [a0([128, 8, 512], BF16) for _ in range(4)]
        sc_bf = a0([128, 8], BF16)
        mod_row = a0([128, 6 * D], F32)
        sc = a0([128, 8], F32)
        fa = Alloc(FREE_BASE, END)
        xt = [fa([128, D], F32) for _ in range(4)]
        xnb = fa([128, NT, D], BF16)
        stt = [fa([128, 2, 6], F32) for _ in range(4)]
        mvt = [fa([128, 2], F32) for _ in range(4)]
        rst = [fa([128, 1], F32) for _ in range(4)]
        wada_v = wada_d.rearrange("(kc p) n -> p kc n", p=128)
        S.dma("sp", lambda e: e.dma_start(out=sc, in_=ccol_d), "cc", writes=["sc"])
        S.dma("sp", lambda e: e.dma_start(out=mod_row[0:1, :], in_=bada_d), "modrow", writes=["modrow"])
        S.act(lambda e: e.activation(sc_bf, sc, AF.Silu), reads=["sc"], writes=["sc_bf"])

        def ln_tile(tt):
            i = tt % 4
            st_, mv_, rs_ = stt[i], mvt[i], rst[i]
            S.dma("act", lambda e: e.dma_start(out=xt[i], in_=x_d[tt * 128:(tt + 1) * 128, :]), "xt%d" % i, writes=["xt%d" % i])
            S.dve(lambda e: e.bn_stats(st_[:, 0, :], xt[i][:, 0:512]), reads=["xt%d" % i], writes=["stt%d" % i])
            S.dve(lambda e: e.bn_stats(st_[:, 1, :], xt[i][:, 512:1024]), reads=["xt%d" % i], writes=["stt%d" % i])
            S.dve(lambda e: e.bn_aggr(mv_, st_), reads=["stt%d" % i], writes=["mvt%d" % i])
            S.act(lambda e: e.activation(rs_, mv_[:, 1:2], AF.Ln, bias=EPS), reads=["mvt%d" % i], writes=["rst%d" % i])
            S.act(lambda e: e.activation(rs_, rs_, AF.Exp, scale=-0.5), reads=["rst%d" % i], writes=["rst%d" % i])
            S.dve(lambda e: e.tensor_scalar(xnb[:, tt, :], xt[i], mv_[:, 0:1], rs_, ALU.subtract, ALU.mult),
                  reads=["xt%d" % i, "mvt%d" % i, "rst%d" % i], writes=["xnb%d" % tt])

        ln_sched = {0: [0, 1], 1: [2], 2: [3], 3: [4, 5], 4: [6], 5: [7], 6: [8, 9], 7: [10], 8: [11], 9: [12, 13], 10: [14], 11: [15]}
        def wada_load(n):
            sl = n % 4
            S.dma("pool", lambda e: e.dma_start(out=wada_s[sl], in_=wada_v[:, :, n * 512:(n + 1) * 512]),
                  "wada%d" % sl, writes=["wada%d" % sl])

        for n in range(4):
            wada_load(n)
        for n in range(12):
            sl = n % 4
            bk = "B%d" % (n % 2)
            for kc in range(8):
                S.pe(lambda e, n=n, sl=sl, kc=kc: e.matmul(banks[n % 2][0:1, :], lhsT=sc_bf[:, kc:kc + 1], rhs=wada_s[sl][:, kc, :],
                                                           start=(kc == 0), stop=(kc == 7)),
                     reads=["sc_bf", "wada%d" % sl], writes=[bk])
            if n + 4 < 12:
                wada_load(n + 4)
            S.dve(lambda e, n=n: e.tensor_tensor(mod_row[0:1, n * 512:(n + 1) * 512], banks[n % 2][0:1, :],
                                                        mod_row[0:1, n * 512:(n + 1) * 512], ALU.add),
                  reads=[bk, "modrow"], writes=["modrow"])
            for tt in ln_sched[n]:
                ln_tile(tt)
        for idx, base in enumerate([1024, 0, 4096, 3072]):
            for ch in range(8):
                S.pe(lambda e, idx=idx, base=base, ch=ch: e.matmul(banks[2][:, idx * 8 + ch:idx * 8 + ch + 1],
                                                                  lhsT=mod_row[0:1, base + ch * 128:base + (ch + 1) * 128],
                                                                  rhs=ones_f[0:1, 0:1], start=True, stop=True),
                     reads=["modrow", "c:ones"], writes=["B2"])
        S.dve(lambda e: e.tensor_copy(modcol, banks[2][:, 0:32]), reads=["B2"], writes=["modcol"])
        S.dve(lambda e: e.tensor_scalar(modcol[:, 0:8], modcol[:, 0:8], 1.0, None, ALU.add), reads=["modcol"], writes=["modcol"])
        S.dve(lambda e: e.tensor_scalar(modcol[:, 16:24], modcol[:, 16:24], 1.0, None, ALU.add), reads=["modcol"], writes=["modcol"])
        for gi, (gbc, base) in enumerate([(gate1_bc, 2048), (gate2_bc, 5120)]):
            for hf in range(2):
                bi = 3
                S.pe(lambda e, bi=bi, base=base, hf=hf: e.matmul(banks[bi], lhsT=ones_f[0:1, :],
                                                                rhs=mod_row[0:1, base + hf * 512:base + (hf + 1) * 512],
                                                                start=True, stop=True),
                     reads=["modrow", "c:ones"], writes=["B%d" % bi])
                S.act(lambda e, gbc=gbc, bi=bi, hf=hf: e.copy(gbc[:, hf * 512:(hf + 1) * 512], banks[bi]),
                      reads=["B%d" % bi], writes=["gatebc%d" % gi])
        S.pool(lambda e: e.tensor_scalar(gate2_bc, gate2_bc, 1.0 / ALPHA, None, ALU.mult), reads=["gatebc1"], writes=["gatebc1"])
        dump("modcol", modcol, ["modcol"])
        dump("gate1", gate1_bc, ["gatebc0"])
        for g in range(NG):
            for kc in range(8):
                bi = 4 + kc % 4
                pb = banks[bi].bitcast(BF16)
                for j in range(4):
                    tt = 4 * g + j
                    S.pe(lambda e, pb=pb, tt=tt, j=j, kc=kc: e.transpose(pb[:, j * 128:(j + 1) * 128],
                                                                        xnb[:, tt, kc * 128:(kc + 1) * 128], ident_bf),
                         reads=["xnb%d" % tt, "c:identbf"], writes=["B%d" % bi])
                if kc % 2 == 0:
                    S.act(lambda e, pb=pb, kc=kc, g=g: e.activation(HT[:, kc, g * 512:(g + 1) * 512], pb[:, 0:512], AF.Identity,
                                                                    bias=modcol[:, 8 + kc:9 + kc], scale=modcol[:, kc:kc + 1]),
                          reads=["B%d" % bi, "modcol"], writes=["HT%d_%d" % (g, kc)])
                else:
                    S.dve(lambda e, pb=pb, kc=kc, g=g: e.tensor_scalar(HT[:, kc, g * 512:(g + 1) * 512], pb[:, 0:512],
                                                                       modcol[:, kc:kc + 1], modcol[:, 8 + kc:9 + kc],
                                                                       ALU.mult, ALU.add),
                          reads=["B%d" % bi, "modcol"], writes=["HT%d_%d" % (g, kc)])
        HTK = ["HT%d_%d" % (g, kc) for g in range(NG) for kc in range(8)]
        if "hT" in dbg_d:
            S.barrier(dummy)
            hdump = carve(ACC_BASE, [128, 8 * 2048], F32)
            S.dve(lambda e: e.tensor_copy(hdump.rearrange("p (a b) -> p a b", a=8), HT), reads=HTK, writes=["hdump"])
            dump("hT", hdump, ["hdump"])
        S.barrier(dummy)

        if upto == "A":
            S.stopped = True
        fb = Alloc(FREE_BASE, END)
        wslot = [fb([128, 8, 512], BF16) for _ in range(3)]
        wctr = [0]

        def load_w(col0, ncols, src=None, nkc=8):
            si = wctr[0] % 3
            wctr[0] += 1
            src = win_v if src is None else src
            dst = wslot[si][:, 0:nkc, 0:ncols]
            S.dma("pool", lambda e: e.dma_start(out=dst, in_=src[:, 0:nkc, col0:col0 + ncols]), "ws%d" % si, writes=["ws%d" % si])
            return dst, "ws%d" % si

        def proj_fm(wap, wkey, g, bank, nkc=8, M=128):
            for kc in range(nkc):
                S.pe(lambda e, kc=kc: e.matmul(banks[bank][0:M, :], lhsT=wap[:, kc, 0:M], rhs=HT[:, kc, g * 512:(g + 1) * 512],
                                               start=(kc == 0), stop=(kc == nkc - 1)),
                     reads=[wkey, "HT%d_%d" % (g, kc)], writes=["B%d" % bank])

        def proj_tm(wap, wkey, tt, bank, ncols, nkc=8):
            for kc in range(nkc):
                S.pe(lambda e, kc=kc: e.matmul(banks[bank][:, 0:ncols], lhsT=HT[:, kc, tt * 128:(tt + 1) * 128], rhs=wap[:, kc, 0:ncols],
                                               start=(kc == 0), stop=(kc == nkc - 1)),
                     reads=[wkey, "HT%d_%d" % (tt // 4, kc)], writes=["B%d" % bank])

        sa = Alloc(ACC_BASE, HT_BASE)
        v_sb = sa([128, NT, 512], BF16)
        qT = [sa([128, T], BF16) for _ in range(2)]
        kT = [sa([128, T], BF16) for _ in range(2)]
        EB = [sa([128, 2, 512], F32) for _ in range(3)]
        SPB = [sa([128, 2, 512], BF16) for _ in range(2)]
        ACB = [sa([128, 2, 512], BF16) for _ in range(2)]
        WTB = [sa([128, 2, 512], BF16) for _ in range(2)]
        NEGM = sa([128, 128], BF16)
        NEGTRI = sa([128, 128], BF16)
        NEGONES = sa([128, 128], BF16)
        ZEROS = sa([128, 512], BF16)
        mtmp = EB[0][:, 0, :]
        S.pool(lambda e: e.memset(mtmp[:, 0:128], 0.0), writes=["mtmp"])
        S.pool(lambda e: e.affine_select(out=mtmp[:, 0:128], in_=mtmp[:, 0:128], pattern=[[1, 128]], compare_op=ALU.is_gt, fill=NEG,
                                         base=0, channel_multiplier=-1), reads=["mtmp"], writes=["mtmp"])
        S.pool(lambda e: e.tensor_copy(NEGM, mtmp[:, 0:128]), reads=["mtmp"], writes=["c:negm"])
        S.pool(lambda e: e.memset(mtmp[:, 0:128], 0.0), reads=["mtmp"], writes=["mtmp"])
        S.pool(lambda e: e.affine_select(out=mtmp[:, 0:128], in_=mtmp[:, 0:128], pattern=[[1, 128]], compare_op=ALU.is_gt, fill=-1.0,
                                         base=0, channel_multiplier=-1), reads=["mtmp"], writes=["mtmp"])
        S.pool(lambda e: e.tensor_copy(NEGTRI, mtmp[:, 0:128]), reads=["mtmp"], writes=["c:negtri"])
        S.pool(lambda e: e.memset(NEGONES, -1.0), writes=["c:negones"])
        S.pool(lambda e: e.memset(ZEROS, 0.0), writes=["c:zeros"])
        S.barrier(dummy)

        wv, wvk = load_w(1024, 512)
        for tt in range(NT):
            bi = 6 + tt % 2
            proj_tm(wv, wvk, tt, bi, 512)
            if tt % 2 == 0:
                S.act(lambda e, tt=tt, bi=bi: e.copy(v_sb[:, tt, :], banks[bi]), reads=["B%d" % bi], writes=["vsb%d" % tt])
            else:
                S.dve(lambda e, tt=tt, bi=bi: e.tensor_copy(v_sb[:, tt, :], banks[bi]), reads=["B%d" % bi], writes=["vsb%d" % tt])

        def qk_units(hp):
            pb2 = hp % 2
            wq, wqk = load_w(hp * 128, 128)
            wk, wkk = load_w(512 + hp * 128, 128)
            units = []
            for g in range(NG):
                def uq(g=g):
                    proj_fm(wq, wqk, g, 7)
                    S.act(lambda e: e.activation(qT[pb2][:, g * 512:(g + 1) * 512], banks[7], AF.Copy, scale=0.125),
                          reads=["B7"], writes=["qT%d_%d" % (pb2, g)])

                def uk(g=g):
                    proj_fm(wk, wkk, g, 7)
                    S.dve(lambda e: e.tensor_copy(kT[pb2][:, g * 512:(g + 1) * 512], banks[7]),
                          reads=["B7"], writes=["kT%d_%d" % (pb2, g)])
                units.append(uq)
                units.append(uk)
            return units

        for u in qk_units(0):
            u()
        for hp in range(4):
            pb_ = hp % 2
            pending = qk_units(hp + 1) if hp + 1 < 4 else []
            blk = [0]
            for s in range(NG):
                nkb = 4 * s + 4
                qkey = "qT%d_%d" % (pb_, s)

                def geom(k):
                    kb = nkb - 1 - k
                    j = kb - 4 * s
                    c0 = j * 128 if j >= 0 else 0
                    return kb, j, c0

                def qk(hh, k):
                    kb, j, c0 = geom(k)
                    hb = hh * 64
                    ksl = kT[pb_][hb:hb + 64, kb * 128:(kb + 1) * 128]
                    qsl = qT[pb_][hb:hb + 64, s * 512 + c0:(s + 1) * 512]
                    return ksl, qsl, "kT%d_%d" % (pb_, kb // 4)

                def emit_Z(k):
                    kb, j, c0 = geom(k)
                    for hh in range(2):
                        zb = 2 * (k % 2) + hh
                        ksl, qsl, kkey = qk(hh, k)
                        S.pe(lambda e, zb=zb, ksl=ksl, qsl=qsl, j=j, c0=c0: e.matmul(banks[zb][:, c0:512], lhsT=ksl, rhs=qsl, start=True, stop=(j < 0)),
                             reads=[kkey, qkey], writes=["B%d" % zb])
                        if j >= 0:
                            S.pe(lambda e, zb=zb, c0=c0: e.matmul(banks[zb][:, c0:c0 + 128], lhsT=ident_bf, rhs=NEGM, start=False, stop=True),
                                 reads=["c:identbf", "c:negm"], writes=["B%d" % zb])

                def emit_E(k):
                    kb, j, c0 = geom(k)
                    p = k % 2
                    for hh in range(2):
                        zb = 2 * p + hh
                        S.act(lambda e, hh=hh, zb=zb: e.activation(EB[k % 3][:, hh, c0:512], banks[zb][:, c0:512], AF.Exp),
                              reads=["B%d" % zb], writes=["E%d_%d" % (hh, k % 3)])

                def emit_SP(k):
                    kb, j, c0 = geom(k)
                    p = k % 2
                    for hh in range(2):
                        S.act(lambda e, hh=hh: e.activation(SPB[p][:, hh, c0:512], EB[k % 3][:, hh, c0:512], AF.Ln, bias=1.0),
                              reads=["E%d_%d" % (hh, k % 3)], writes=["SP%d_%d" % (hh, p)])

                def emit_L(k):
                    kb, j, c0 = geom(k)
                    p = k % 2
                    for hh in range(2):
                        lb = 4 + hh
                        S.pe(lambda e, lb=lb, hh=hh: e.matmul(banks[lb][:, c0:512], lhsT=NEGTRI, rhs=SPB[p][:, hh, c0:512], start=True, stop=(k == 0)),
                             reads=["c:negtri", "SP%d_%d" % (hh, p)], writes=["B%d" % lb])
                        if k > 0:
                            S.pe(lambda e, lb=lb, hh=hh: e.matmul(banks[lb][:, c0:512], lhsT=NEGONES, rhs=ACB[p][:, hh, c0:512], start=False, stop=True),
                                 reads=["c:negones", "acb%d_%d" % (hh, p)], writes=["B%d" % lb])

                def emit_W(k):
                    kb, j, c0 = geom(k)
                    p = k % 2
                    for hh in range(2):
                        S.act(lambda e, hh=hh: e.activation(WTB[p][:, hh, c0:512], banks[4 + hh][:, c0:512], AF.Exp),
                              reads=["B%d" % (4 + hh)], writes=["WT%d_%d" % (hh, p)])

                def emit_M(k):
                    kb, j, c0 = geom(k)
                    p = k % 2
                    for hh in range(2):
                        S.dve(lambda e, hh=hh: e.tensor_tensor(WTB[p][:, hh, c0:512], WTB[p][:, hh, c0:512], EB[k % 3][:, hh, c0:512], ALU.mult),
                              reads=["WT%d_%d" % (hh, p), "E%d_%d" % (hh, k % 3)], writes=["WT%d_%d" % (hh, p)])

                def emit_ACC(k):
                    kb, j, c0 = geom(k)
                    p = k % 2
                    for hh in range(2):
                        if c0 > 0:
                            S.pool(lambda e, hh=hh: e.memset(ACB[1 - p][:, hh, 0:c0], 0.0), writes=["acb%d_%d" % (hh, 1 - p)])
                        S.dve(lambda e, hh=hh: e.tensor_tensor(ACB[1 - p][:, hh, c0:512], ACB[p][:, hh, c0:512], SPB[p][:, hh, c0:512], ALU.add),
                              reads=["acb%d_%d" % (hh, p), "SP%d_%d" % (hh, p)], writes=["acb%d_%d" % (hh, 1 - p)])

                def emit_PV(k):
                    kb, j, c0 = geom(k)
                    p = k % 2
                    for hh in range(2):
                        hb = hh * 64
                        ob = 6
                        vcol = hp * 128 + hb
                        S.pe(lambda e, ob=ob, hb=hb, kb=kb, vcol=vcol, hh=hh, p=p, c0=c0, k=k, nkb=nkb: e.matmul(
                            banks[ob][hb:hb + 64, c0:512], lhsT=v_sb[:, kb, vcol:vcol + 64], rhs=WTB[p][:, hh, c0:512], start=False, stop=(k == nkb - 1)),
                            reads=["vsb%d" % kb, "WT%d_%d" % (hh, p)], writes=["B%d" % ob])

                S.pool(lambda e: e.memset(ACB[0], 0.0), writes=["acb0_0", "acb1_0"])
                for hh in range(2):
                    hb = hh * 64
                    S.pe(lambda e, hh=hh, hb=hb: e.matmul(banks[6][hb:hb + 64, :], lhsT=ZEROS[:, 0:64], rhs=ZEROS, start=True, stop=False),
                         reads=["c:zeros"], writes=["B6"])
                emit_Z(0)
                emit_Z(1)
                emit_E(0)
                emit_Z(2)
                emit_SP(0)
                emit_E(1)
                for k in range(nkb):
                    emit_L(k)
                    if k >= 1:
                        emit_PV(k - 1)
                    if k + 3 < nkb:
                        emit_Z(k + 3)
                    if k + 1 < nkb:
                        emit_SP(k + 1)
                    if k + 2 < nkb:
                        emit_E(k + 2)
                    emit_W(k)
                    if k + 1 < nkb:
                        emit_ACC(k)
                    emit_M(k)
                    blk[0] += 1
                    if pending and blk[0] % 4 == 0:
                        pending.pop(0)()
                emit_PV(nkb - 1)
                S.dve(lambda e, s=s, hp=hp: e.tensor_copy(OSB[:, hp, s * 512:(s + 1) * 512], banks[6]), reads=["B6"], writes=["OSB"])
            for u in pending:
                u()
        if "osbT" in dbg_d:
            S.barrier(dummy)
            odump = carve(FREE_BASE + 24576, [128, 4 * 2048], F32)
            S.dve(lambda e: e.tensor_copy(odump.rearrange("p (a b) -> p a b", a=4), OSB), reads=["OSB"], writes=["odump"])
            dump("osbT", odump, ["odump"])
        S.barrier(dummy)


        if upto == "SB":
            S.stopped = True
        ga = Alloc(ACC_BASE, HT_BASE)
        glrT = ga([128, T], F32)
        wup_aug = ga([128, 256], F32)
        qgT = ga([128, T], BF16)
        kgT = ga([128, T], BF16)
        kg_tm = ga([128, NT, 128], BF16)
        vg_tm = ga([128, NT, 256], BF16)
        rT = ga([128, 2, T], BF16)
        eg_all = ga([128, T], F32)
        eng_b = [ga([128, 128], F32) for _ in range(3)]
        ed_b = [ga([128, 128], F32) for _ in range(3)]
        scb = [[ga([128, 128], BF16) for _ in range(2)] for _ in range(2)]
        sq_b = [[ga([128, 128], F32) for _ in range(2)] for _ in range(2)]
        oraw = [[ga([128, 128], F32) for _ in range(2)] for _ in range(2)]
        rs_b = [[ga([128, 128], F32) for _ in range(2)] for _ in range(2)]
        tn_b = [[ga([128, 128], F32) for _ in range(2)] for _ in range(2)]
        S_f = ga([128, 128], F32)
        S_b = [ga([128, 128], BF16) for _ in range(2)]
        normw = ga([128, 4], F32)
        etm = [ga([128, 256], F32) for _ in range(2)]
        gf = Alloc(FREE_BASE + 24576, END)
        sp_tm = gf([128, NT, 256], F32)
        TRI_I = gf([128, 128], F32)
        TRI_A = gf([128, 128], F32)
        M2 = gf([128, 128], F32)
        S.pool(lambda e: e.memset(TRI_I, -1.0 / 16.0), writes=["c:trii"])
        S.pool(lambda e: e.affine_select(out=TRI_I, in_=TRI_I, pattern=[[1, 128]], compare_op=ALU.is_ge, fill=0.0, base=0,
                                         channel_multiplier=-1), reads=["c:trii"], writes=["c:trii"])
        S.pool(lambda e: e.memset(TRI_I[0:64, 64:128], 0.0), reads=["c:trii"], writes=["c:trii"])
        S.pool(lambda e: e.memset(TRI_A, -1.0 / 16.0), writes=["c:tria"])
        S.pool(lambda e: e.affine_select(out=TRI_A, in_=TRI_A, pattern=[[-1, 128]], compare_op=ALU.is_gt, fill=0.0, base=0,
                                         channel_multiplier=1), reads=["c:tria"], writes=["c:tria"])
        S.pool(lambda e: e.memset(TRI_A[64:128, 0:64], 0.0), reads=["c:tria"], writes=["c:tria"])
        S.pool(lambda e: e.memset(M2, 1.0), writes=["c:m2"])
        S.pool(lambda e: e.affine_select(out=M2, in_=M2, pattern=[[1, 128]], compare_op=ALU.is_ge, fill=0.0, base=0,
                                         channel_multiplier=-1), reads=["c:m2"], writes=["c:m2"])
        S.pool(lambda e: e.memset(M2[0:64, 64:128], 0.0), reads=["c:m2"], writes=["c:m2"])
        S.pool(lambda e: e.memset(glrT[0:32, :], 1.0), writes=["glrT"])
        S.dma("sp", lambda e: e.dma_start(out=wup_aug[0:16, :], in_=wup_d), "wup", writes=["wup"])
        S.dma("sp", lambda e: e.dma_start(out=wup_aug[16:17, :], in_=bgate_d), "wup", writes=["wup"])
        S.dma("sp", lambda e: e.dma_start(out=normw, in_=normw_d), "normw", writes=["normw"])
        wglr, wglrk = load_w(3072, 16)
        for g in range(NG):
            proj_fm(wglr, wglrk, g, 6, M=16)
            S.act(lambda e, g=g: e.copy(glrT[0:16, g * 512:(g + 1) * 512], banks[6][0:16, :]), reads=["B6"], writes=["glrT"])
        for tt in range(NT):
            p = tt % 2
            S.pe(lambda e, p=p, tt=tt: e.matmul(banks[p][:, 0:256], lhsT=glrT[0:17, tt * 128:(tt + 1) * 128], rhs=wup_aug[0:17, :],
                                                start=True, stop=True), reads=["glrT", "wup"], writes=["B%d" % p])
            S.act(lambda e, p=p: e.activation(etm[p], banks[p][:, 0:256], AF.Exp, scale=-1.0), reads=["B%d" % p], writes=["etm%d" % p])
            S.act(lambda e, p=p, tt=tt: e.activation(sp_tm[:, tt, :], etm[p], AF.Ln, bias=1.0), reads=["etm%d" % p], writes=["sptm%d" % tt])
        for gp in range(2):
            wqg, wqgk = load_w(1536 + gp * 128, 128)
            wkg, wkgk = load_w(1792 + gp * 128, 128)
            wvg, wvgk = load_w(2048 + gp * 256, 256)
            for g in range(NG):
                tk = [4 * g + j for j in range(4)]
                proj_fm(wqg, wqgk, g, 6)
                S.act(lambda e, g=g: e.activation(qgT[:, g * 512:(g + 1) * 512], banks[6], AF.Copy, scale=0.125),
                      reads=["B6"], writes=["qg%d" % t for t in tk])
                proj_fm(wkg, wkgk, g, 7)
                S.dve(lambda e, g=g: e.tensor_copy(kgT[:, g * 512:(g + 1) * 512], banks[7]), reads=["B7"], writes=["kg%d" % t for t in tk])
            for tt in range(NT):
                proj_tm(wkg, wkgk, tt, 6, 128)
                S.act(lambda e, tt=tt: e.copy(kg_tm[:, tt, :], banks[6][:, 0:128]), reads=["B6"], writes=["kgtm%d" % tt])
                proj_tm(wvg, wvgk, tt, 7, 256)
                S.dve(lambda e, tt=tt: e.tensor_copy(vg_tm[:, tt, :], banks[7][:, 0:256]), reads=["B7"], writes=["vgtm%d" % tt])
            wrg, wrgk = load_w(2560 + gp * 256, 256)
            for hh in range(2):
                for g in range(NG):
                    proj_fm(wrg[:, :, hh * 128:(hh + 1) * 128], wrgk, g, 6 + g % 2)
                    S.act(lambda e, g=g, hh=hh: e.activation(rT[:, hh, g * 512:(g + 1) * 512], banks[6 + g % 2], AF.Silu),
                          reads=["B%d" % (6 + g % 2)], writes=["rT%d_%d" % (hh, g)])
            S.pool(lambda e: e.memset(S_f, 0.0), writes=["S_f"])
            S.pool(lambda e: e.memset(S_b[1], 0.0), writes=["S_b1"])

            def stA(tt):
                p3 = tt % 3
                cs = slice(tt * 128, (tt + 1) * 128)
                spk = "sptm%d" % tt
                spsl = sp_tm[:, tt, gp * 128:(gp + 1) * 128]
                S.pe(lambda e: e.matmul(banks[0][:, 0:128], lhsT=spsl, rhs=TRI_I, start=True, stop=True), reads=[spk, "c:trii"], writes=["B0"])
                S.pe(lambda e: e.matmul(banks[1][:, 0:128], lhsT=TRI_A, rhs=spsl, start=True, stop=True), reads=[spk, "c:tria"], writes=["B1"])
                S.act(lambda e: e.activation(eg_all[:, cs], banks[0][:, 0:128], AF.Exp), reads=["B0"], writes=["eg%d" % tt])
                S.act(lambda e: e.activation(eng_b[p3], banks[0][:, 0:128], AF.Exp, scale=-1.0), reads=["B0"], writes=["eng%d" % p3])
                S.act(lambda e: e.activation(ed_b[p3], banks[1][:, 0:128], AF.Exp), reads=["B1"], writes=["ed%d" % p3])
                S.dve(lambda e: e.tensor_tensor(qgT[:, cs], qgT[:, cs], eg_all[:, cs], ALU.mult), reads=["qg%d" % tt, "eg%d" % tt], writes=["qg%d" % tt])
                S.pool(lambda e: e.tensor_tensor(kgT[:, cs], kgT[:, cs], eng_b[p3], ALU.mult), reads=["kg%d" % tt, "eng%d" % p3], writes=["kg%d" % tt])
                S.dve(lambda e: e.tensor_tensor(kg_tm[:, tt, :], kg_tm[:, tt, :], ed_b[p3], ALU.mult), reads=["kgtm%d" % tt, "ed%d" % p3], writes=["kgtm%d" % tt])

            def stB_sc(tt, hh):
                p = tt % 2
                cs = slice(tt * 128, (tt + 1) * 128)
                hb = hh * 64
                S.pe(lambda e: e.matmul(banks[2][:, 0:128], lhsT=kgT[hb:hb + 64, cs], rhs=qgT[hb:hb + 64, cs], start=True, stop=True),
                     reads=["kg%d" % tt, "qg%d" % tt], writes=["B2"])
                S.dve(lambda e: e.tensor_tensor(scb[p][hh], banks[2][:, 0:128], M2, ALU.mult), reads=["B2", "c:m2"], writes=["scb%d_%d" % (p, hh)])

            def stB_intra(tt, hh):
                p = tt % 2
                ob = 3 + 2 * p + hh
                S.pe(lambda e: e.matmul(banks[ob][:, 0:128], lhsT=vg_tm[:, tt, hh * 128:(hh + 1) * 128], rhs=scb[p][hh], start=True, stop=False),
                     reads=["vgtm%d" % tt, "scb%d_%d" % (p, hh)], writes=["B%d" % ob])

            def stC_inter(tt, ch):
                p = tt % 2
                sprev = 1 - ch
                for hh in range(2):
                    hb = hh * 64
                    ob = 3 + 2 * p + hh
                    S.pe(lambda e, ob=ob, hb=hb: e.matmul(banks[ob][:, ch * 64:ch * 64 + 64], lhsT=S_b[sprev][hb:hb + 64, :],
                                                          rhs=qgT[hb:hb + 64, tt * 128 + ch * 64:tt * 128 + ch * 64 + 64], start=False, stop=(ch == 1)),
                         reads=["S_b%d" % sprev, "qg%d" % tt], writes=["B%d" % ob])

            def stC_U(tt, ch):
                rows = slice(ch * 64, ch * 64 + 64)
                for hh in range(2):
                    hb = hh * 64
                    S.pe(lambda e, hb=hb, hh=hh: e.matmul(banks[7][hb:hb + 64, 0:128], lhsT=kg_tm[rows, tt, hb:hb + 64],
                                                          rhs=vg_tm[rows, tt, hh * 128:(hh + 1) * 128], start=True, stop=True),
                         reads=["kgtm%d" % tt, "vgtm%d" % tt], writes=["B7"])
                col = tt * 128 + ch * 64 + 63
                S.dve(lambda e: e.scalar_tensor_tensor(S_f, S_f, eg_all[:, col:col + 1], banks[7][:, 0:128], ALU.mult, ALU.add),
                      reads=["S_f", "eg%d" % tt, "B7"], writes=["S_f"])
                S.act(lambda e: e.copy(S_b[ch], S_f), reads=["S_f"], writes=["S_b%d" % ch])

            def stP_pre(tt):
                p = tt % 2
                for hh in range(2):
                    ob = 3 + 2 * p + hh
                    S.dve(lambda e, hh=hh, ob=ob: e.tensor_copy(oraw[p][hh], banks[ob][:, 0:128]), reads=["B%d" % ob], writes=["oraw%d_%d" % (p, hh)])
                for hh in range(2):
                    S.act(lambda e, hh=hh: e.activation(sq_b[p][hh], oraw[p][hh], AF.Square), reads=["oraw%d_%d" % (p, hh)], writes=["sq%d_%d" % (p, hh)])

            def stP_ms(tt, hh):
                p = tt % 2
                S.pe(lambda e: e.matmul(banks[hh][:, 0:128], lhsT=onesdiv_f, rhs=sq_b[p][hh], start=True, stop=True),
                     reads=["sq%d_%d" % (p, hh), "c:onesdiv"], writes=["B%d" % hh])

            def stP_fin(tt):
                p = tt % 2
                cs = slice(tt * 128, (tt + 1) * 128)
                for hh in range(2):
                    S.act(lambda e, hh=hh: e.activation(rs_b[p][hh], banks[hh][:, 0:128], AF.Ln, bias=EPS), reads=["B%d" % hh], writes=["rs%d_%d" % (p, hh)])
                for hh in range(2):
                    S.act(lambda e, hh=hh: e.activation(rs_b[p][hh], rs_b[p][hh], AF.Exp, scale=-0.5), reads=["rs%d_%d" % (p, hh)], writes=["rs%d_%d" % (p, hh)])
                for hh in range(2):
                    S.dve(lambda e, hh=hh: e.tensor_tensor(tn_b[p][hh], oraw[p][hh], rs_b[p][hh], ALU.mult),
                          reads=["oraw%d_%d" % (p, hh), "rs%d_%d" % (p, hh)], writes=["tn%d_%d" % (p, hh)])
                for hh in range(2):
                    hd = gp * 2 + hh
                    S.dve(lambda e, hh=hh, hd=hd: e.scalar_tensor_tensor(OG[:, hd, cs], tn_b[p][hh], normw[:, hd:hd + 1], rT[:, hh, cs], ALU.mult, ALU.mult),
                          reads=["tn%d_%d" % (p, hh), "normw", "rT%d_%d" % (hh, tt // 4)], writes=["OG"])

            stA(0)
            stA(1)
            stB_sc(0, 0)
            stB_sc(0, 1)
            stB_intra(0, 0)
            stB_intra(0, 1)
            for tt in range(NT):
                nx = tt + 1 < NT
                if tt + 2 < NT:
                    stA(tt + 2)
                stC_inter(tt, 0)
                stC_U(tt, 0)
                if nx:
                    stB_sc(tt + 1, 0)
                if tt >= 1:
                    stP_ms(tt - 1, 0)
                if nx:
                    stB_sc(tt + 1, 1)
                if tt >= 1:
                    stP_ms(tt - 1, 1)
                if nx:
                    stB_intra(tt + 1, 0)
                stC_inter(tt, 1)
                stC_U(tt, 1)
                if nx:
                    stB_intra(tt + 1, 1)
                stP_pre(tt)
                if tt >= 1:
                    stP_fin(tt - 1)
            stP_ms(NT - 1, 0)
            stP_ms(NT - 1, 1)
            stP_fin(NT - 1)
        if "ogT" in dbg_d:
            S.barrier(dummy)
            gdump = carve(FREE_BASE + 24576, [128, 4 * 2048], F32)
            S.dve(lambda e: e.tensor_copy(gdump.rearrange("p (a b) -> p a b", a=4), OG), reads=["OG"], writes=["gdump"])
            dump("ogT", gdump, ["gdump"])
        S.barrier(dummy)


        if upto == "GLA":
            S.stopped = True
        GATES = carve(END - 4096, [128, NT, E], F32)
        c1 = Alloc(FREE_BASE, END - 4096)
        yT = c1([128, NG, 8 * 512], BF16)
        wsb_c = [c1([128, 4, 128], BF16) for _ in range(2)]
        wgl_c = [c1([128, 4, 128], BF16) for _ in range(2)]
        wm1_c = [c1([128, 8, 128], BF16) for _ in range(2)]
        wm2_c = [c1([128, 8, 128], BF16) for _ in range(2)]
        s1b = [c1([128, 512], BF16) for _ in range(2)]
        s2b = [c1([128, 512], BF16) for _ in range(2)]
        t1b = [c1([128, 512], BF16) for _ in range(2)]
        t2b = [c1([128, 512], BF16) for _ in range(2)]
        wsb_v = wsb_d.rearrange("(kc p) n -> p kc n", p=128)
        wgl_v = wgla_d.rearrange("(kc p) n -> p kc n", p=128)
        def c1_load(oc):
            q = oc % 2
            ocs = slice(oc * 128, (oc + 1) * 128)
            S.dma("pool", lambda e: e.dma_start(out=wsb_c[q], in_=wsb_v[:, :, ocs]), "wsbc%d" % q, writes=["wsbc%d" % q])
            S.dma("pool", lambda e: e.dma_start(out=wgl_c[q], in_=wgl_v[:, :, ocs]), "wglc%d" % q, writes=["wglc%d" % q])
            S.dma("pool", lambda e: e.dma_start(out=wm1_c[q], in_=win_v[:, :, 3088 + oc * 128:3088 + (oc + 1) * 128]),
                  "wm1c%d" % q, writes=["wm1c%d" % q])
            S.dma("pool", lambda e: e.dma_start(out=wm2_c[q], in_=win_v[:, :, 4112 + oc * 128:4112 + (oc + 1) * 128]),
                  "wm2c%d" % q, writes=["wm2c%d" % q])

        c1_load(0)
        for oc in range(8):
            q = oc % 2
            if oc + 1 < 8:
                c1_load(oc + 1)
            for g in range(NG):
                r = g % 2
                bA, bB, bM1, bM2 = 4 * r, 4 * r + 1, 4 * r + 2, 4 * r + 3
                gs = slice(g * 512, (g + 1) * 512)
                for kc in range(4):
                    S.pe(lambda e, bA=bA, q=q, kc=kc, gs=gs: e.matmul(banks[bA], lhsT=wsb_c[q][:, kc, :], rhs=OSB[:, kc, gs], start=(kc == 0), stop=(kc == 3)),
                         reads=["wsbc%d" % q, "OSB"], writes=["B%d" % bA])
                for kc in range(4):
                    S.pe(lambda e, bB=bB, q=q, kc=kc, gs=gs: e.matmul(banks[bB], lhsT=wgl_c[q][:, kc, :], rhs=OG[:, kc, gs], start=(kc == 0), stop=(kc == 3)),
                         reads=["wglc%d" % q, "OG"], writes=["B%d" % bB])
                for kc in range(8):
                    S.pe(lambda e, bM1=bM1, q=q, kc=kc, gs=gs: e.matmul(banks[bM1], lhsT=wm1_c[q][:, kc, :], rhs=HT[:, kc, gs], start=(kc == 0), stop=(kc == 7)),
                         reads=["wm1c%d" % q, "HT%d_%d" % (g, kc)], writes=["B%d" % bM1])
                for kc in range(8):
                    S.pe(lambda e, bM2=bM2, q=q, kc=kc, gs=gs: e.matmul(banks[bM2], lhsT=wm2_c[q][:, kc, :], rhs=HT[:, kc, gs], start=(kc == 0), stop=(kc == 7)),
                         reads=["wm2c%d" % q, "HT%d_%d" % (g, kc)], writes=["B%d" % bM2])
                S.act(lambda e, r=r, bM1=bM1: e.activation(s1b[r], banks[bM1], AF.Sigmoid), reads=["B%d" % bM1], writes=["s1b%d" % r])
                S.act(lambda e, r=r, bM2=bM2: e.activation(s2b[r], banks[bM2], AF.Sigmoid), reads=["B%d" % bM2], writes=["s2b%d" % r])
                S.dve(lambda e, r=r, bA=bA: e.tensor_tensor(t1b[r], banks[bA], s1b[r], ALU.mult), reads=["B%d" % bA, "s1b%d" % r], writes=["t1b%d" % r])
                S.dve(lambda e, r=r, bB=bB: e.tensor_tensor(t2b[r], banks[bB], s2b[r], ALU.mult), reads=["B%d" % bB, "s2b%d" % r], writes=["t2b%d" % r])
                S.pool(lambda e, r=r, g=g, oc=oc: e.tensor_tensor(yT[:, g, oc * 512:(oc + 1) * 512], t1b[r], t2b[r], ALU.add),
                       reads=["t1b%d" % r, "t2b%d" % r], writes=["yT%d" % g])
        S.barrier(dummy)

        if upto == "C1":
            S.stopped = True
        c2 = Alloc(OSB_BASE, FREE_BASE)
        wout_s = c2([128, 8, D], BF16)
        xt2 = [c2([128, D], F32) for _ in range(2)]
        xnf = [c2([128, D], F32) for _ in range(2)]
        c2b = Alloc(FREE_BASE + 32768, END - 4096)
        h2f = [c2b([128, 8, 128], F32) for _ in range(2)]
        wr_s = c2b([128, 8, E], F32)
        LOG_all = c2b([128, NT, E], F32)
        rb_bc = c2b([128, E], F32)
        stt2 = [c2b([128, 2, 6], F32) for _ in range(2)]
        mvt2 = [c2b([128, 2], F32) for _ in range(2)]
        rst2 = [c2b([128, 1], F32) for _ in range(2)]
        nmr2 = [c2b([128, 1], F32) for _ in range(2)]
        wout_v = wout_d.rearrange("(kc p) n -> p kc n", p=128)
        for hf in range(2):
            S.dma("pool", lambda e, hf=hf: e.dma_start(out=wout_s[:, :, hf * 512:(hf + 1) * 512], in_=wout_v[:, :, hf * 512:(hf + 1) * 512]),
                  "wout%d" % hf, writes=["wout%d" % hf])
        S.dma("sp", lambda e: e.dma_start(out=wr_s, in_=wr_d.rearrange("(kc p) n -> p kc n", p=128)), "wr", writes=["wr"])
        S.dma("sp", lambda e: e.dma_start(out=rb_bc, in_=rb_d.partition_broadcast(128)), "rb", writes=["rb"])
        S.dma("sp", lambda e: e.dma_start(out=lnbc[:, 0, :], in_=ln1g_d.partition_broadcast(128)), "lnbc", writes=["lnbc"])
        S.dma("sp", lambda e: e.dma_start(out=lnbc[:, 1, :], in_=ln1b_d.partition_broadcast(128)), "lnbc", writes=["lnbc"])
        for hf in range(2):
            for kc in range(8):
                S.dve(lambda e, kc=kc, hf=hf: e.tensor_tensor(wout_s[:, kc, hf * 512:(hf + 1) * 512], wout_s[:, kc, hf * 512:(hf + 1) * 512],
                                                              gate1_bc[:, hf * 512:(hf + 1) * 512], ALU.mult),
                      reads=["wout%d" % hf, "gatebc0"], writes=["wout%d" % hf])

        def ln_stat(src, i, rkey, eps):
            st_, mv_, rs_ = stt2[i], mvt2[i], rst2[i]
            S.dve(lambda e: e.bn_stats(st_[:, 0, :], src[:, 0:512]), reads=[rkey], writes=["stt%d" % i])
            S.dve(lambda e: e.bn_stats(st_[:, 1, :], src[:, 512:1024]), reads=[rkey], writes=["stt%d" % i])
            S.dve(lambda e: e.bn_aggr(mv_, st_), reads=["stt%d" % i], writes=["mvt%d" % i])
            S.act(lambda e: e.activation(rs_, mv_[:, 1:2], AF.Ln, bias=eps), reads=["mvt%d" % i], writes=["rst%d" % i])
            S.act(lambda e: e.activation(rs_, rs_, AF.Exp, scale=-0.5), reads=["rst%d" % i], writes=["rst%d" % i])

        def ln_nmr(i):
            S.dve(lambda e: e.tensor_scalar(nmr2[i], mvt2[i][:, 0:1], rst2[i], -1.0, ALU.mult, ALU.mult),
                  reads=["mvt%d" % i, "rst%d" % i], writes=["nmr%d" % i])

        def X1(tt):
            g, jt = tt // 4, tt % 4
            i = tt % 2
            S.dma("sp", lambda e: e.dma_start(out=xt2[i], in_=x_d[tt * 128:(tt + 1) * 128, :]), "xt2_%d" % i, writes=["xt2_%d" % i])
            for hf in range(2):
                bk = 2 * i + hf
                for kc in range(8):
                    S.pe(lambda e, bk=bk, hf=hf, kc=kc: e.matmul(banks[bk], lhsT=yT[:, g, kc * 512 + jt * 128:kc * 512 + (jt + 1) * 128],
                                                                rhs=wout_s[:, kc, hf * 512:(hf + 1) * 512], start=(kc == 0), stop=(kc == 7)),
                         reads=["yT%d" % g, "wout%d" % hf], writes=["B%d" % bk])
                S.dve(lambda e, bk=bk, hf=hf: e.scalar_tensor_tensor(xt2[i][:, hf * 512:(hf + 1) * 512], xt2[i][:, hf * 512:(hf + 1) * 512], ALPHA,
                                                                    banks[bk], ALU.mult, ALU.add),
                      reads=["xt2_%d" % i, "B%d" % bk], writes=["xt2_%d" % i])
            ln_stat(xt2[i], i, "xt2_%d" % i, EPS)

        def Y1a(tt):
            i = tt % 2
            ln_nmr(i)
            S.act(lambda e: e.activation(xnf[i], xt2[i], AF.Identity, bias=nmr2[i], scale=rst2[i]),
                  reads=["xt2_%d" % i, "rst%d" % i, "nmr%d" % i], writes=["xnf%d" % i])

        def Y1b(tt):
            i = tt % 2
            S.pool(lambda e: e.tensor_tensor(xnf[i], xnf[i], lnbc[:, 0, :], ALU.mult), reads=["xnf%d" % i, "lnbc"], writes=["xnf%d" % i])
            S.pool(lambda e: e.tensor_tensor(ACC[:, tt, :], xnf[i], lnbc[:, 1, :], ALU.add), reads=["xnf%d" % i, "lnbc"], writes=["acc%d" % tt])

        X1(0)
        for tt in range(NT):
            Y1a(tt)
            if tt + 1 < NT:
                X1(tt + 1)
            Y1b(tt)

        def Y2a(tt):
            i = tt % 2
            ln_nmr(i)
            S.act(lambda e: e.activation(xnf[i], ACC[:, tt, :], AF.Identity, bias=nmr2[i], scale=rst2[i]),
                  reads=["acc%d" % tt, "rst%d" % i, "nmr%d" % i], writes=["xnf%d" % i])
            for kc in range(8):
                bi = 4 + 2 * i + kc // 4
                S.pe(lambda e, bi=bi, kc=kc: e.transpose(banks[bi][:, (kc % 4) * 128:(kc % 4 + 1) * 128], xnf[i][:, kc * 128:(kc + 1) * 128], ident_f),
                     reads=["xnf%d" % i, "c:identf"], writes=["B%d" % bi])

        def Y2b(tt):
            i = tt % 2
            for kc in range(8):
                bi = 4 + 2 * i + kc // 4
                src = banks[bi][:, (kc % 4) * 128:(kc % 4 + 1) * 128]
                if kc < 4:
                    S.act(lambda e, src=src, kc=kc: e.activation(h2f[i][:, kc, :], src, AF.Identity, bias=modcol[:, 24 + kc:25 + kc], scale=modcol[:, 16 + kc:17 + kc]),
                          reads=["B%d" % bi, "modcol"], writes=["h2f%d_%d" % (i, kc)])
                else:
                    S.dve(lambda e, src=src, kc=kc: e.tensor_scalar(h2f[i][:, kc, :], src, modcol[:, 16 + kc:17 + kc], modcol[:, 24 + kc:25 + kc], ALU.mult, ALU.add),
                          reads=["B%d" % bi, "modcol"], writes=["h2f%d_%d" % (i, kc)])

        def Z2a(tt):
            i = tt % 2
            for kc in range(8):
                S.pe(lambda e, kc=kc: e.matmul(banks[i][:, 0:E], lhsT=h2f[i][:, kc, :], rhs=wr_s[:, kc, :], start=(kc == 0), stop=(kc == 7)),
                     reads=["h2f%d_%d" % (i, kc), "wr"], writes=["B%d" % i])

        def Z2b(tt):
            i = tt % 2
            h2k = ["h2f%d_%d" % (i, kc) for kc in range(8)]
            S.pool(lambda e: e.tensor_copy(HT[:, :, tt * 128:(tt + 1) * 128], h2f[i]), reads=h2k, writes=["h2T%d" % tt])
            S.dve(lambda e: e.tensor_copy(LOG_all[:, tt, :], banks[i][:, 0:E]), reads=["B%d" % i], writes=["logits"])

        ln_stat(ACC[:, 0, :], 0, "acc0", EPS)
        for tt in range(NT):
            if tt >= 1:
                Z2a(tt - 1)
            Y2a(tt)
            if tt + 1 < NT:
                ln_stat(ACC[:, tt + 1, :], (tt + 1) % 2, "acc%d" % (tt + 1), EPS)
            if tt >= 1:
                Z2b(tt - 1)
            Y2b(tt)
        Z2a(NT - 1)
        Z2b(NT - 1)
        if "h2T" in dbg_d:
            S.barrier(dummy)
            h2dump = carve(OSB_BASE, [128, 8 * 2048], F32)
            S.dve(lambda e: e.tensor_copy(h2dump.rearrange("p (a b) -> p a b", a=8), HT), reads=[], writes=["h2dump"])
            dump("h2T", h2dump, ["h2dump"])
        S.barrier(dummy)
        def emit_pass3():
            c3 = Alloc(FREE_BASE + 8192, END - 4096)
            sc_all = c3([128, NT, E], F32)
            sel_all = c3([128, NT, E], F32)
            selm_all = c3([128, NT * 8, 8], F32)
            m8a = c3([128, NT * 8, 8], F32)
            gs_all = c3([128, NT * 8], F32)
            gm8_all = c3([128, NT, 8], F32)
            gmask_all = c3([128, NT * 8], F32)
            gneg_all = c3([128, NT * 8], F32)
            e8_all = c3([128, NT, 8], F32)
            cho_all = c3([128, NT, E], F32)
            wsum_all = c3([128, NT], F32)
            wrec_all = c3([128, NT], F32)
            S.act(lambda e: e.activation(sc_all, LOG_all, AF.Sigmoid), reads=["logits"], writes=["sc_all"])
            for tt in range(NT):
                S.dve(lambda e, tt=tt: e.tensor_tensor(sel_all[:, tt, :], sc_all[:, tt, :], rb_bc, ALU.add), reads=["sc_all", "rb"], writes=["sel%d" % tt])
            sel_g = sel_all.rearrange("p a (g k) -> p (a g) k", k=8)
            for tt in range(NT):
                for gi in range(8):
                    S.dve(lambda e, tt=tt, gi=gi: e.max(out=m8a[:, tt * 8 + gi, :], in_=sel_g[:, tt * 8 + gi, :]), reads=["sel%d" % tt], writes=["m8a%d_%d" % (tt, gi)])
            S.dve(lambda e: e.tensor_tensor(gs_all, m8a[:, :, 0], m8a[:, :, 1], ALU.add),
                  reads=["m8a%d_%d" % (t, gi) for t in range(NT) for gi in range(8)], writes=["gs_all"])
            for tt in range(NT):
                S.dve(lambda e, tt=tt: e.max(out=gm8_all[:, tt, :], in_=gs_all[:, tt * 8:(tt + 1) * 8]), reads=["gs_all"], writes=["gm8_%d" % tt])
            for tt in range(NT):
                S.dve(lambda e, tt=tt: e.tensor_scalar(gmask_all[:, tt * 8:(tt + 1) * 8], gs_all[:, tt * 8:(tt + 1) * 8], gm8_all[:, tt, 3:4], None, ALU.is_ge),
                      reads=["gs_all", "gm8_%d" % tt], writes=["gmask%d" % tt])
            gmk = ["gmask%d" % t for t in range(NT)]
            S.dve(lambda e: e.tensor_scalar(gneg_all, gmask_all, 1e30, -1e30, ALU.mult, ALU.add), reads=gmk, writes=["gneg"])
            S.dve(lambda e: e.tensor_tensor(selm_all, sel_g, gmask_all.unsqueeze(2).broadcast_to([128, NT * 8, 8]), ALU.mult),
                  reads=gmk + ["sel%d" % t for t in range(NT)], writes=["selm"])
            S.dve(lambda e: e.tensor_tensor(selm_all, selm_all, gneg_all.unsqueeze(2).broadcast_to([128, NT * 8, 8]), ALU.add),
                  reads=["selm", "gneg"], writes=["selm"])
            selm_t = selm_all.rearrange("p (a g) k -> p a (g k)", g=8)
            for tt in range(NT):
                S.dve(lambda e, tt=tt: e.max(out=e8_all[:, tt, :], in_=selm_t[:, tt, :]), reads=["selm"], writes=["e8_%d" % tt])
            for tt in range(NT):
                S.dve(lambda e, tt=tt: e.tensor_scalar(cho_all[:, tt, :], selm_t[:, tt, :], e8_all[:, tt, 7:8], None, ALU.is_ge),
                      reads=["selm", "e8_%d" % tt], writes=["cho%d" % tt])
            chk = ["cho%d" % t for t in range(NT)]
            S.dve(lambda e: e.tensor_tensor(cho_all, cho_all, sc_all, ALU.mult), reads=chk + ["sc_all"], writes=chk)
            S.dve(lambda e: e.reduce_sum(wsum_all, cho_all, AX.X), reads=chk, writes=["wsum"])
            S.dve(lambda e: e.reciprocal(wrec_all, wsum_all), reads=["wsum"], writes=["wrec"])
            for tt in range(NT):
                S.dve(lambda e, tt=tt: e.tensor_scalar(GATES[:, tt, :], cho_all[:, tt, :], wrec_all[:, tt:tt + 1], 2.5, ALU.mult, ALU.mult),
                      reads=["cho%d" % tt, "wrec"], writes=["gates%d" % tt])
            if "gates" in dbg_d:
                dump("gates", GATES.rearrange("p a b -> p (a b)"), ["gates%d" % t for t in range(NT)])

        if "x1" in dbg_d:
            dump("x1", ACC.rearrange("p a b -> p (a b)"), ["acc%d" % t for t in range(NT)])
        if upto == "C2":
            S.stopped = True
        da = Alloc(OSB_BASE, END - 4096)
        wgu_s = [da([128, 8, 512], BF16) for _ in range(2)]
        wd_s = [da([128, 2, D], BF16) for _ in range(2)]
        sgb = [da([128, 512], BF16) for _ in range(2)]
        actT = [da([128, 2, 512], BF16) for _ in range(2)]
        otile = [da([128, D], F32) for _ in range(2)]
        stt3 = [da([128, 2, 6], F32) for _ in range(2)]
        mvt3 = [da([128, 2], F32) for _ in range(2)]
        rst3 = [da([128, 1], F32) for _ in range(2)]
        S.dma("sp", lambda e: e.dma_start(out=lnbc[:, 0, :], in_=ln2g_d.partition_broadcast(128)), "lnbc", writes=["lnbc"])
        S.dma("sp", lambda e: e.dma_start(out=lnbc[:, 1, :], in_=ln2b_d.partition_broadcast(128)), "lnbc", writes=["lnbc"])
        nmr3 = [da([128, 1], F32) for _ in range(2)]

        def emit_final(tt):
            i = tt % 2
            ak = ["acc%d_0" % tt, "acc%d_1" % tt]
            S.dve(lambda e: e.bn_stats(stt3[i][:, 0, :], ACC[:, tt, 0:512]), reads=ak, writes=["stt%d" % i])
            S.dve(lambda e: e.bn_stats(stt3[i][:, 1, :], ACC[:, tt, 512:1024]), reads=ak, writes=["stt%d" % i])
            S.dve(lambda e: e.bn_aggr(mvt3[i], stt3[i]), reads=["stt%d" % i], writes=["mvt%d" % i])
            S.act(lambda e: e.activation(rst3[i], mvt3[i][:, 1:2], AF.Ln, bias=EPS / (ALPHA * ALPHA)), reads=["mvt%d" % i], writes=["rst%d" % i])
            S.act(lambda e: e.activation(rst3[i], rst3[i], AF.Exp, scale=-0.5), reads=["rst%d" % i], writes=["rst%d" % i])
            S.dve(lambda e: e.tensor_scalar(nmr3[i], mvt3[i][:, 0:1], rst3[i], -1.0, ALU.mult, ALU.mult), reads=["mvt%d" % i, "rst%d" % i], writes=["nmr%d" % i])
            S.act(lambda e: e.activation(otile[i], ACC[:, tt, :], AF.Identity, bias=nmr3[i], scale=rst3[i]),
                  reads=ak + ["rst%d" % i, "nmr%d" % i], writes=["otile%d" % i])
            S.dve(lambda e: e.tensor_tensor(otile[i], otile[i], lnbc[:, 0, :], ALU.mult), reads=["otile%d" % i, "lnbc"], writes=["otile%d" % i])
            S.pool(lambda e: e.tensor_tensor(otile[i], otile[i], lnbc[:, 1, :], ALU.add), reads=["otile%d" % i, "lnbc"], writes=["otile%d" % i])
            S.dma("sp", lambda e: e.dma_start(out=out_d[tt * 128:(tt + 1) * 128, :], in_=otile[i]), "out%d" % i, reads=["otile%d" % i])

        order = [E] + list(range(E))

        def moe_load(pos):
            ex = order[pos]
            q = pos % 2
            if ex < E:
                gu_v = wgu_d[ex].rearrange("(kc p) f -> p kc f", p=128)
                dn_v = wd_d[ex].rearrange("(j p) o -> p j o", p=128)
            else:
                gu_v = wsgu_d.rearrange("(kc p) f -> p kc f", p=128)
                dn_v = wsd_d.rearrange("(j p) o -> p j o", p=128)
            for hf in range(2):
                S.dma("pool", lambda e, hf=hf: e.dma_start(out=wgu_s[q][:, hf * 4:(hf + 1) * 4, :], in_=gu_v[:, hf * 4:(hf + 1) * 4, :]),
                      "wgu%d" % q, writes=["wgu%d" % q])
            S.dma("pool", lambda e: e.dma_start(out=wd_s[q], in_=dn_v), "wd%d" % q, writes=["wd%d" % q])
            for j in range(2):
                S.pool(lambda e, j=j: e.tensor_tensor(wd_s[q][:, j, :], wd_s[q][:, j, :], gate2_bc, ALU.mult),
                       reads=["wd%d" % q, "gatebc1"], writes=["wd%d" % q])

        def moe_g_or_u(idx, j, which):
            pos, g = idx // NG, idx % NG
            q = pos % 2
            gs = slice(g * 512, (g + 1) * 512)
            hk = ["h2T%d" % t for t in range(4 * g, 4 * g + 4)]
            bk = j if which == 0 else 2 + j
            c0 = j * 128 if which == 0 else 256 + j * 128
            for kc in range(8):
                S.pe(lambda e, kc=kc: e.matmul(banks[bk], lhsT=wgu_s[q][:, kc, c0:c0 + 128], rhs=HT[:, kc, gs],
                                               start=(kc == 0), stop=(kc == 7)), reads=["wgu%d" % q] + hk, writes=["B%d" % bk])

        def moe_act(idx, j):
            a = idx % 2
            S.act(lambda e: e.activation(sgb[j], banks[j], AF.Silu), reads=["B%d" % j], writes=["sgb%d" % j])
            S.dve(lambda e: e.tensor_tensor(actT[a][:, j, :], banks[2 + j], sgb[j], ALU.mult),
                  reads=["B%d" % (2 + j), "sgb%d" % j], writes=["actT%d_%d" % (a, j)])

        def moe_down_tile(idx, jt):
            pos, g = idx // NG, idx % NG
            ex = order[pos]
            q = pos % 2
            a = idx % 2
            tt = 4 * g + jt
            for hf in range(2):
                bD = 4 + (jt * 2 + hf) % 4
                for j in range(2):
                    S.pe(lambda e, bD=bD, j=j, hf=hf: e.matmul(banks[bD], lhsT=actT[a][:, j, jt * 128:(jt + 1) * 128],
                                                               rhs=wd_s[q][:, j, hf * 512:(hf + 1) * 512], start=(j == 0), stop=(j == 1)),
                         reads=["actT%d_%d" % (a, j), "wd%d" % q], writes=["B%d" % bD])
                accs = ACC[:, tt, hf * 512:(hf + 1) * 512]
                sca = GATES[:, tt, ex:ex + 1] if ex < E else 1.0
                S.dve(lambda e, bD=bD, accs=accs, sca=sca: e.scalar_tensor_tensor(accs, banks[bD], sca, accs, ALU.mult, ALU.add),
                      reads=["B%d" % bD, "acc%d_%d" % (tt, hf), "gates%d" % tt], writes=["acc%d_%d" % (tt, hf)])
            if pos == E and jt == 3:
                for t4 in range(4):
                    emit_final(4 * g + t4)

        nidx = (E + 1) * NG
        for idx in range(nidx + 1):
            pos, g = idx // NG, idx % NG
            live = idx < nidx
            prev = idx - 1 if idx >= 1 else None
            if live and g == 0:
                if pos == 1:
                    emit_pass3()
                moe_load(pos)
            if live:
                moe_g_or_u(idx, 0, 0)
            if prev is not None:
                moe_down_tile(prev, 0)
            if live:
                moe_g_or_u(idx, 0, 1)
                moe_act(idx, 0)
            if prev is not None:
                moe_down_tile(prev, 1)
            if live:
                moe_g_or_u(idx, 1, 0)
            if prev is not None:
                moe_down_tile(prev, 2)
            if live:
                moe_g_or_u(idx, 1, 1)
                moe_act(idx, 1)
            if prev is not None:
                moe_down_tile(prev, 3)
        if "acc" in dbg_d:
            dump("acc", ACC.rearrange("p a b -> p (a b)"), ["acc%d_%d" % (t, h) for t in range(NT) for h in range(2)])
        S.emit(final_keys=["out0", "out1"] + (["dbg"] if dbg_d else []))
    return nc


def _in_maps(inputs):
    f = lambda a: np.ascontiguousarray(np.asarray(a, dtype=np.float32))
    shared = {
        "w_ada": f(inputs["w_ada"][0]), "b_ada": f(inputs["b_ada"][0]).reshape(1, -1),
        "w_in": f(inputs["w_in"][0]), "gla_w_gate_up": f(inputs["gla_w_gate_up"][0]),
        "gla_b_gate": f(inputs["gla_b_gate"][0]).reshape(1, -1),
        "gla_norm_w_col": f(np.asarray(inputs["gla_norm_w"][0]).reshape(4, 128).T),
        "w_branch_sb": f(inputs["w_branch_sb"][0]), "w_branch_gla": f(inputs["w_branch_gla"][0]),
        "w_out": f(inputs["w_out"][0]), "ln1_g": f(inputs["ln1_g"][0]).reshape(1, -1), "ln1_b": f(inputs["ln1_b"][0]).reshape(1, -1),
        "w_router": f(inputs["w_router"][0]), "router_bias": f(inputs["router_bias"][0]).reshape(1, -1),
        "w_exp_gate_up": f(inputs["w_exp_gate_up"][0]), "w_exp_down": f(inputs["w_exp_down"][0]),
        "w_shared_gate_up": f(inputs["w_shared_gate_up"][0]), "w_shared_down": f(inputs["w_shared_down"][0]),
        "ln2_g": f(inputs["ln2_g"][0]).reshape(1, -1), "ln2_b": f(inputs["ln2_b"][0]).reshape(1, -1),
    }
    maps = []
    for b in range(8):
        m = dict(shared)
        m["x"] = f(inputs["x"][b])
        m["c_col"] = f(np.asarray(inputs["c"][b]).reshape(8, 128).T)
        maps.append(m)
    return maps


def kernel(**inputs):
    nc = build()
    maps = _in_maps(inputs)
    res = run_bass_kernel_spmd(nc, maps, core_ids=list(range(8)))
    return np.stack([np.asarray(r["out"], dtype=np.float32) for r in res.results], axis=0)
```

```python
import numpy as np
from contextlib import ExitStack
import concourse.bass as bass
import concourse.mybir as mybir
from concourse.bass_utils import run_bass_kernel_spmd

F32 = mybir.dt.float32
BF16 = mybir.dt.bfloat16
F32R = mybir.dt.float32r
AF = mybir.ActivationFunctionType
ALU = mybir.AluOpType
AX = mybir.AxisListType

T = 2048
D = 1024
NT = 16
NG = 4
E = 64
ALPHA = 2.0 ** 0.25
EPS = 1e-5
NEG = -30000.0
ENGS = ("pe", "act", "dve", "pool", "sp")


class _Op:
    __slots__ = ("eng", "fn", "deps", "is_dma", "dma_sem", "token", "signal")


class Sched:
    def __init__(self, nc, stack):
        self.nc = nc
        self.stack = stack
        self.ops = []
        self.last_w = {}
        self.readers = {}
        self.dma_sems = {}
        self.dma_cnt = {}
        self.stopped = False

    def _mk(self, eng, fn, reads, writes):
        op = _Op()
        if self.stopped:
            return op
        op.eng = eng
        op.fn = fn
        op.is_dma = False
        op.dma_sem = None
        op.signal = False
        op.token = None
        deps = set()
        reads = list(reads) + ["PHASE"]
        for r in reads:
            w = self.last_w.get(r)
            if w is not None:
                deps.add(w)
        for r in writes:
            w = self.last_w.get(r)
            if w is not None:
                deps.add(w)
            for rd in self.readers.get(r, ()):
                deps.add(rd)
        op.deps = deps
        i = len(self.ops)
        self.ops.append(op)
        for r in reads:
            if r.startswith("c:"):
                continue
            self.readers.setdefault(r, []).append(i)
        for r in writes:
            self.last_w[r] = i
            self.readers[r] = []
        return op

    def add(self, eng, fn, reads=(), writes=()):
        self._mk(eng, fn, reads, writes)

    def pe(self, fn, reads=(), writes=()):
        self._mk("pe", fn, reads, writes)

    def act(self, fn, reads=(), writes=()):
        self._mk("act", fn, reads, writes)

    def dve(self, fn, reads=(), writes=()):
        self._mk("dve", fn, reads, writes)

    def pool(self, fn, reads=(), writes=()):
        self._mk("pool", fn, reads, writes)

    def dma(self, queue, fn, semkey, reads=(), writes=()):
        op = self._mk(queue, fn, reads, writes)
        if self.stopped:
            return
        op.is_dma = True
        if semkey not in self.dma_sems:
            self.dma_sems[semkey] = self.stack.enter_context(self.nc.semaphore("d_" + semkey))
            self.dma_cnt[semkey] = 0
        self.dma_cnt[semkey] += 16
        op.dma_sem = self.dma_sems[semkey]
        op.token = (semkey, self.dma_cnt[semkey])
        op.signal = True

    def barrier(self, dummy):
        op = self._mk("pool", lambda e: e.memset(dummy, 0.0), [], ["PHASE"])
        return op

    def emit(self, final_keys=()):
        nc = self.nc
        ops = self.ops

        def need_sync(p, op):
            if p.is_dma or op.is_dma:
                return True
            if p.eng != op.eng:
                return True
            return p.eng != "pe"

        for op in ops:
            for d in op.deps:
                p = ops[d]
                if not p.is_dma and need_sync(p, op):
                    p.signal = True
        cnt = {e: 0 for e in ENGS}
        for op in ops:
            if not op.is_dma and op.signal:
                cnt[op.eng] += 1
                op.token = (op.eng, cnt[op.eng])
        eng_sem = {e: self.stack.enter_context(nc.semaphore("e_" + e)) for e in ENGS}
        per_eng = {e: [] for e in ENGS}
        for op in ops:
            per_eng[op.eng].append(op)

        def semof(k):
            return eng_sem[k] if k in eng_sem else self.dma_sems[k]

        def run(name, eng):
            waited = {}
            for op in per_eng[name]:
                need = {}
                for d in op.deps:
                    p = ops[d]
                    if not need_sync(p, op):
                        continue
                    k, v = p.token
                    if need.get(k, 0) < v:
                        need[k] = v
                for k, v in need.items():
                    if waited.get(k, 0) >= v:
                        continue
                    eng.wait_ge(semof(k), v)
                    waited[k] = v
                ins = op.fn(eng)
                if op.is_dma:
                    ins.then_inc(op.dma_sem, 16)
                elif op.signal:
                    ins.then_inc(eng_sem[op.eng], 1)
            if name == "sp":
                for k in final_keys:
                    if k in self.dma_sems:
                        eng.wait_ge(self.dma_sems[k], self.dma_cnt[k])

        with nc.Block() as block:
            @block.tensor
            def _(e):
                run("pe", e)

            @block.scalar
            def _(e):
                run("act", e)

            @block.vector
            def _(e):
                run("dve", e)

            @block.gpsimd
            def _(e):
                run("pool", e)

            @block.sync
            def _(e):
                run("sp", e)


def build(dbg=None, upto=None):
    dbg = dbg or {}
    nc = bass.Bass("TRN2", target_bir_lowering=False)

    def din(name, shape):
        return nc.dram_tensor(name, list(shape), F32, kind="ExternalInput").ap()

    x_d = din("x", [T, D])
    ccol_d = din("c_col", [128, 8])
    wada_d = din("w_ada", [D, 6 * D])
    bada_d = din("b_ada", [1, 6 * D])
    win_d = din("w_in", [D, 5136])
    wup_d = din("gla_w_gate_up", [16, 256])
    bgate_d = din("gla_b_gate", [1, 256])
    normw_d = din("gla_norm_w_col", [128, 4])
    wsb_d = din("w_branch_sb", [512, D])
    wgla_d = din("w_branch_gla", [512, D])
    wout_d = din("w_out", [D, D])
    ln1g_d = din("ln1_g", [1, D])
    ln1b_d = din("ln1_b", [1, D])
    wr_d = din("w_router", [D, E])
    rb_d = din("router_bias", [1, E])
    wgu_d = din("w_exp_gate_up", [E, D, 512])
    wd_d = din("w_exp_down", [E, 256, D])
    wsgu_d = din("w_shared_gate_up", [D, 512])
    wsd_d = din("w_shared_down", [256, D])
    ln2g_d = din("ln2_g", [1, D])
    ln2b_d = din("ln2_b", [1, D])
    out_d = nc.dram_tensor("out", [T, D], F32, kind="ExternalOutput").ap()
    dbg_d = {k: nc.dram_tensor("dbg_" + k, list(shp), F32, kind="ExternalOutput").ap() for k, shp in dbg.items()}

    with ExitStack() as st:
        ARW = 212000 // 4
        AR = st.enter_context(nc.sbuf_tensor("arena", [128, ARW], F32))
        PS = st.enter_context(nc.psum_tensor("psum_all", [128, 8 * 512], F32))[:]
        banks = [PS[:, i * 512:(i + 1) * 512] for i in range(8)]

        def bank2(b0):
            return PS[:, b0 * 512:(b0 + 2) * 512].rearrange("p (b n) -> p b n", b=2)
        S = Sched(nc, st)

        def carve(off, shape, dt):
            n = int(np.prod(shape[1:]))
            nb = n * (4 if dt == F32 else 2)
            assert off % 4 == 0 and nb % 4 == 0, (off, nb)
            assert off + nb <= ARW * 4, ("arena overflow", off, nb)
            a = AR[:, off // 4:(off + nb) // 4]
            if dt != F32:
                a = a.bitcast(dt)
            if len(shape) == 3:
                a = a.rearrange("p (a b) -> p a b", a=shape[1])
            return a

        class Alloc:
            def __init__(self, base, limit):
                self.off = base
                self.limit = limit

            def __call__(self, shape, dt):
                n = int(np.prod(shape[1:])) * (4 if dt == F32 else 2)
                n = (n + 31) // 32 * 32
                a = carve(self.off, shape, dt)
                self.off += n
                assert self.off <= self.limit, ("region overflow", self.off, self.limit)
                return a

        P_BASE, ACC_BASE, HT_BASE, OSB_BASE, OG_BASE, FREE_BASE, END = 0, 18432, 83968, 116736, 133120, 149504, 212000
        pa = Alloc(P_BASE, ACC_BASE)
        ident_bf = pa([128, 128], BF16)
        ident_f = pa([128, 128], F32)
        ones_f = pa([128, 128], F32)
        onesdiv_f = pa([128, 128], F32)
        modcol = pa([128, 32], F32)
        gate1_bc = pa([128, D], F32)
        gate2_bc = pa([128, D], F32)
        lnbc = pa([128, 2, D], F32)
        dummy = pa([128, 8], F32)
        ACC = carve(ACC_BASE, [128, NT, D], F32)
        HT = carve(HT_BASE, [128, 8, T], BF16)
        OSB = carve(OSB_BASE, [128, 4, T], BF16)
        OG = carve(OG_BASE, [128, 4, T], BF16)

        win_v = win_d.rearrange("(kc p) n -> p kc n", p=128)

        def dump(name, ap, reads):
            if name in dbg_d:
                S.dma("sp", lambda e: e.dma_start(out=dbg_d[name], in_=ap), "dbg", reads=reads)

        S.pool(lambda e: e.memset(ident_f, 0.0), writes=["c:identf"])
        S.pool(lambda e: e.affine_select(out=ident_f, in_=ident_f, pattern=[[-1, 128]], compare_op=ALU.not_equal,
                                         fill=1.0, base=0, channel_multiplier=1), reads=["c:identf"], writes=["c:identf"])
        S.pool(lambda e: e.tensor_copy(ident_bf, ident_f), reads=["c:identf"], writes=["c:identbf"])
        S.pool(lambda e: e.memset(ones_f, 1.0), writes=["c:ones"])
        S.pool(lambda e: e.memset(onesdiv_f, 1.0 / 128.0), writes=["c:onesdiv"])

        a0 = Alloc(ACC_BASE, HT_BASE)
        wada_s = [a0([128, 8, 512], BF16) for _ in range(4)]
        sc_bf = a0([128, 8], BF16)
        mod_row = a0([128, 6 * D], F32)
        sc = a0([128, 8], F32)
        fa = Alloc(FREE_BASE, END)
        xt = [fa([128, D], F32) for _ in range(4)]
        xnb = fa([128, NT, D], BF16)
        stt = [fa([128, 2, 6], F32) for _ in range(4)]
        mvt = [fa([128, 2], F32) for _ in range(4)]
        rst = [fa([128, 1], F32) for _ in range(4)]
        wada_v = wada_d.rearrange("(kc p) n -> p kc n", p=128)
        S.dma("sp", lambda e: e.dma_start(out=sc, in_=ccol_d), "cc", writes=["sc"])
        S.dma("sp", lambda e: e.dma_start(out=mod_row[0:1, :], in_=bada_d), "modrow", writes=["modrow"])
        S.act(lambda e: e.activation(sc_bf, sc, AF.Silu), reads=["sc"], writes=["sc_bf"])

        def ln_tile(tt):
            i = tt % 4
            st_, mv_, rs_ = stt[i], mvt[i], rst[i]
            S.dma("act", lambda e: e.dma_start(out=xt[i], in_=x_d[tt * 128:(tt + 1) * 128, :]), "xt%d" % i, writes=["xt%d" % i])
            S.dve(lambda e: e.bn_stats(st_[:, 0, :], xt[i][:, 0:512]), reads=["xt%d" % i], writes=["stt%d" % i])
            S.dve(lambda e: e.bn_stats(st_[:, 1, :], xt[i][:, 512:1024]), reads=["xt%d" % i], writes=["stt%d" % i])
            S.dve(lambda e: e.bn_aggr(mv_, st_), reads=["stt%d" % i], writes=["mvt%d" % i])
            S.act(lambda e: e.activation(rs_, mv_[:, 1:2], AF.Ln, bias=EPS), reads=["mvt%d" % i], writes=["rst%d" % i])
            S.act(lambda e: e.activation(rs_, rs_, AF.Exp, scale=-0.5), reads=["rst%d" % i], writes=["rst%d" % i])
            S.dve(lambda e: e.tensor_scalar(xnb[:, tt, :], xt[i], mv_[:, 0:1], rs_, ALU.subtract, ALU.mult),
                  reads=["xt%d" % i, "mvt%d" % i, "rst%d" % i], writes=["xnb%d" % tt])

        ln_sched = {0: [0, 1], 1: [2], 2: [3], 3: [4, 5], 4: [6], 5: [7], 6: [8, 9], 7: [10], 8: [11], 9: [12, 13], 10: [14], 11: [15]}
        def wada_load(n):
            sl = n % 4
            S.dma("pool", lambda e: e.dma_start(out=wada_s[sl], in_=wada_v[:, :, n * 512:(n + 1) * 512]),
                  "wada%d" % sl, writes=["wada%d" % sl])

        for n in range(4):
            wada_load(n)
        for n in range(12):
            sl = n % 4
            bk = "B%d" % (n % 2)
            for kc in range(8):
                S.pe(lambda e, n=n, sl=sl, kc=kc: e.matmul(banks[n % 2][0:1, :], lhsT=sc_bf[:, kc:kc + 1], rhs=wada_s[sl][:, kc, :],
                                                           start=(kc == 0), stop=(kc == 7)),
                     reads=["sc_bf", "wada%d" % sl], writes=[bk])
            if n + 4 < 12:
                wada_load(n + 4)
            S.dve(lambda e, n=n: e.tensor_tensor(mod_row[0:1, n * 512:(n + 1) * 512], banks[n % 2][0:1, :],
                                                        mod_row[0:1, n * 512:(n + 1) * 512], ALU.add),
                  reads=[bk, "modrow"], writes=["modrow"])
            for tt in ln_sched[n]:
                ln_tile(tt)
        for idx, base in enumerate([1024, 0, 4096, 3072]):
            for ch in range(8):
                S.pe(lambda e, idx=idx, base=base, ch=ch: e.matmul(banks[2][:, idx * 8 + ch:idx * 8 + ch + 1],
                                                                  lhsT=mod_row[0:1, base + ch * 128:base + (ch + 1) * 128],
                                                                  rhs=ones_f[0:1, 0:1], start=True, stop=True),
                     reads=["modrow", "c:ones"], writes=["B2"])
        S.dve(lambda e: e.tensor_copy(modcol, banks[2][:, 0:32]), reads=["B2"], writes=["modcol"])
        S.dve(lambda e: e.tensor_scalar(modcol[:, 0:8], modcol[:, 0:8], 1.0, None, ALU.add), reads=["modcol"], writes=["modcol"])
        S.dve(lambda e: e.tensor_scalar(modcol[:, 16:24], modcol[:, 16:24], 1.0, None, ALU.add), reads=["modcol"], writes=["modcol"])
        for gi, (gbc, base) in enumerate([(gate1_bc, 2048), (gate2_bc, 5120)]):
            for hf in range(2):
                bi = 3
                S.pe(lambda e, bi=bi, base=base, hf=hf: e.matmul(banks[bi], lhsT=ones_f[0:1, :],
                                                                rhs=mod_row[0:1, base + hf * 512:base + (hf + 1) * 512],
                                                                start=True, stop=True),
                     reads=["modrow", "c:ones"], writes=["B%d" % bi])
                S.act(lambda e, gbc=gbc, bi=bi, hf=hf: e.copy(gbc[:, hf * 512:(hf + 1) * 512], banks[bi]),
                      reads=["B%d" % bi], writes=["gatebc%d" % gi])
        S.pool(lambda e: e.tensor_scalar(gate2_bc, gate2_bc, 1.0 / ALPHA, None, ALU.mult), reads=["gatebc1"], writes=["gatebc1"])
        dump("modcol", modcol, ["modcol"])
        dump("gate1", gate1_bc, ["gatebc0"])
        for g in range(NG):
            for kc in range(8):
                bi = 4 + kc % 4
                pb = banks[bi].bitcast(BF16)
                for j in range(4):
                    tt = 4 * g + j
                    S.pe(lambda e, pb=pb, tt=tt, j=j, kc=kc: e.transpose(pb[:, j * 128:(j + 1) * 128],
                                                                        xnb[:, tt, kc * 128:(kc + 1) * 128], ident_bf),
                         reads=["xnb%d" % tt, "c:identbf"], writes=["B%d" % bi])
                if kc % 2 == 0:
                    S.act(lambda e, pb=pb, kc=kc, g=g: e.activation(HT[:, kc, g * 512:(g + 1) * 512], pb[:, 0:512], AF.Identity,
                                                                    bias=modcol[:, 8 + kc:9 + kc], scale=modcol[:, kc:kc + 1]),
                          reads=["B%d" % bi, "modcol"], writes=["HT%d_%d" % (g, kc)])
                else:
                    S.dve(lambda e, pb=pb, kc=kc, g=g: e.tensor_scalar(HT[:, kc, g * 512:(g + 1) * 512], pb[:, 0:512],
                                                                       modcol[:, kc:kc + 1], modcol[:, 8 + kc:9 + kc],
                                                                       ALU.mult, ALU.add),
                          reads=["B%d" % bi, "modcol"], writes=["HT%d_%d" % (g, kc)])
        HTK = ["HT%d_%d" % (g, kc) for g in range(NG) for kc in range(8)]
        if "hT" in dbg_d:
            S.barrier(dummy)
            hdump = carve(ACC_BASE, [128, 8 * 2048], F32)
            S.dve(lambda e: e.tensor_copy(hdump.rearrange("p (a b) -> p a b", a=8), HT), reads=HTK, writes=["hdump"])
            dump("hT", hdump, ["hdump"])
        S.barrier(dummy)

        if upto == "A":
            S.stopped = True
        fb = Alloc(FREE_BASE, END)
        wslot = [fb([128, 8, 512], BF16) for _ in range(3)]
        wctr = [0]

        def load_w(col0, ncols, src=None, nkc=8):
            si = wctr[0] % 3
            wctr[0] += 1
            src = win_v if src is None else src
            dst = wslot[si][:, 0:nkc, 0:ncols]
            S.dma("pool", lambda e: e.dma_start(out=dst, in_=src[:, 0:nkc, col0:col0 + ncols]), "ws%d" % si, writes=["ws%d" % si])
            return dst, "ws%d" % si

        def proj_fm(wap, wkey, g, bank, nkc=8, M=128):
            for kc in range(nkc):
                S.pe(lambda e, kc=kc: e.matmul(banks[bank][0:M, :], lhsT=wap[:, kc, 0:M], rhs=HT[:, kc, g * 512:(g + 1) * 512],
                                               start=(kc == 0), stop=(kc == nkc - 1)),
                     reads=[wkey, "HT%d_%d" % (g, kc)], writes=["B%d" % bank])

        def proj_tm(wap, wkey, tt, bank, ncols, nkc=8):
            for kc in range(nkc):
                S.pe(lambda e, kc=kc: e.matmul(banks[bank][:, 0:ncols], lhsT=HT[:, kc, tt * 128:(tt + 1) * 128], rhs=wap[:, kc, 0:ncols],
                                               start=(kc == 0), stop=(kc == nkc - 1)),
                     reads=[wkey, "HT%d_%d" % (tt // 4, kc)], writes=["B%d" % bank])

        sa = Alloc(ACC_BASE, HT_BASE)
        v_sb = sa([128, NT, 512], BF16)
        qT = [sa([128, T], BF16) for _ in range(2)]
        kT = [sa([128, T], BF16) for _ in range(2)]
        EB = [sa([128, 2, 512], F32) for _ in range(3)]
        SPB = [sa([128, 2, 512], BF16) for _ in range(2)]
        ACB = [sa([128, 2, 512], BF16) for _ in range(2)]
        WTB = [sa([128, 2, 512], BF16) for _ in range(2)]
        NEGM = sa([128, 128], BF16)
        NEGTRI = sa([128, 128], BF16)
        NEGONES = sa([128, 128], BF16)
        ZEROS = sa([128, 512], BF16)
        mtmp = EB[0][:, 0, :]
        S.pool(lambda e: e.memset(mtmp[:, 0:128], 0.0), writes=["mtmp"])
        S.pool(lambda e: e.affine_select(out=mtmp[:, 0:128], in_=mtmp[:, 0:128], pattern=[[1, 128]], compare_op=ALU.is_gt, fill=NEG,
                                         base=0, channel_multiplier=-1), reads=["mtmp"], writes=["mtmp"])
        S.pool(lambda e: e.tensor_copy(NEGM, mtmp[:, 0:128]), reads=["mtmp"], writes=["c:negm"])
        S.pool(lambda e: e.memset(mtmp[:, 0:128], 0.0), reads=["mtmp"], writes=["mtmp"])
        S.pool(lambda e: e.affine_select(out=mtmp[:, 0:128], in_=mtmp[:, 0:128], pattern=[[1, 128]], compare_op=ALU.is_gt, fill=-1.0,
                                         base=0, channel_multiplier=-1), reads=["mtmp"], writes=["mtmp"])
        S.pool(lambda e: e.tensor_copy(NEGTRI, mtmp[:, 0:128]), reads=["mtmp"], writes=["c:negtri"])
        S.pool(lambda e: e.memset(NEGONES, -1.0), writes=["c:negones"])
        S.pool(lambda e: e.memset(ZEROS, 0.0), writes=["c:zeros"])
        S.barrier(dummy)

        wv, wvk = load_w(1024, 512)
        for tt in range(NT):
            bi = 6 + tt % 2
            proj_tm(wv, wvk, tt, bi, 512)
            if tt % 2 == 0:
                S.act(lambda e, tt=tt, bi=bi: e.copy(v_sb[:, tt, :], banks[bi]), reads=["B%d" % bi], writes=["vsb%d" % tt])
            else:
                S.dve(lambda e, tt=tt, bi=bi: e.tensor_copy(v_sb[:, tt, :], banks[bi]), reads=["B%d" % bi], writes=["vsb%d" % tt])

        def qk_units(hp):
            pb2 = hp % 2
            wq, wqk = load_w(hp * 128, 128)
            wk, wkk = load_w(512 + hp * 128, 128)
            units = []
            for g in range(NG):
                def uq(g=g):
                    proj_fm(wq, wqk, g, 7)
                    S.act(lambda e: e.activation(qT[pb2][:, g * 512:(g + 1) * 512], banks[7], AF.Copy, scale=0.125),
                          reads=["B7"], writes=["qT%d_%d" % (pb2, g)])

                def uk(g=g):
                    proj_fm(wk, wkk, g, 7)
                    S.dve(lambda e: e.tensor_copy(kT[pb2][:, g * 512:(g + 1) * 512], banks[7]),
                          reads=["B7"], writes=["kT%d_%d" % (pb2, g)])
                units.append(uq)
                units.append(uk)
            return units

        for u in qk_units(0):
            u()
        for hp in range(4):
            pb_ = hp % 2
            pending = qk_units(hp + 1) if hp + 1 < 4 else []
            blk = [0]
            for s in range(NG):
                nkb = 4 * s + 4
                qkey = "qT%d_%d" % (pb_, s)

                def geom(k):
                    kb = nkb - 1 - k
                    j = kb - 4 * s
                    c0 = j * 128 if j >= 0 else 0
                    return kb, j, c0

                def qk(hh, k):
                    kb, j, c0 = geom(k)
                    hb = hh * 64
                    ksl = kT[pb_][hb:hb + 64, kb * 128:(kb + 1) * 128]
                    qsl = qT[pb_][hb:hb + 64, s * 512 + c0:(s + 1) * 512]
                    return ksl, qsl, "kT%d_%d" % (pb_, kb // 4)

                def emit_Z(k):
                    kb, j, c0 = geom(k)
                    for hh in range(2):
                        zb = 2 * (k % 2) + hh
                        ksl, qsl, kkey = qk(hh, k)
                        S.pe(lambda e, zb=zb, ksl=ksl, qsl=qsl, j=j, c0=c0: e.matmul(banks[zb][:, c0:512], lhsT=ksl, rhs=qsl, start=True, stop=(j < 0)),
                             reads=[kkey, qkey], writes=["B%d" % zb])
                        if j >= 0:
                            S.pe(lambda e, zb=zb, c0=c0: e.matmul(banks[zb][:, c0:c0 + 128], lhsT=ident_bf, rhs=NEGM, start=False, stop=True),
                                 reads=["c:identbf", "c:negm"], writes=["B%d" % zb])

                def emit_E(k):
                    kb, j, c0 = geom(k)
                    p = k % 2
                    for hh in range(2):
                        zb = 2 * p + hh
                        S.act(lambda e, hh=hh, zb=zb: e.activation(EB[k % 3][:, hh, c0:512], banks[zb][:, c0:512], AF.Exp),
                              reads=["B%d" % zb], writes=["E%d_%d" % (hh, k % 3)])

                def emit_SP(k):
                    kb, j, c0 = geom(k)
                    p = k % 2
                    for hh in range(2):
                        S.act(lambda e, hh=hh: e.activation(SPB[p][:, hh, c0:512], EB[k % 3][:, hh, c0:512], AF.Ln, bias=1.0),
                              reads=["E%d_%d" % (hh, k % 3)], writes=["SP%d_%d" % (hh, p)])

                def emit_L(k):
                    kb, j, c0 = geom(k)
                    p = k % 2
                    for hh in range(2):
                        lb = 4 + hh
                        S.pe(lambda e, lb=lb, hh=hh: e.matmul(banks[lb][:, c0:512], lhsT=NEGTRI, rhs=SPB[p][:, hh, c0:512], start=True, stop=(k == 0)),
                             reads=["c:negtri", "SP%d_%d" % (hh, p)], writes=["B%d" % lb])
                        if k > 0:
                            S.pe(lambda e, lb=lb, hh=hh: e.matmul(banks[lb][:, c0:512], lhsT=NEGONES, rhs=ACB[p][:, hh, c0:512], start=False, stop=True),
                                 reads=["c:negones", "acb%d_%d" % (hh, p)], writes=["B%d" % lb])

                def emit_W(k):
                    kb, j, c0 = geom(k)
                    p = k % 2
                    for hh in range(2):
                        S.act(lambda e, hh=hh: e.activation(WTB[p][:, hh, c0:512], banks[4 + hh][:, c0:512], AF.Exp),
                              reads=["B%d" % (4 + hh)], writes=["WT%d_%d" % (hh, p)])

                def emit_M(k):
                    kb, j, c0 = geom(k)
                    p = k % 2
                    for hh in range(2):
                        S.dve(lambda e, hh=hh: e.tensor_tensor(WTB[p][:, hh, c0:512], WTB[p][:, hh, c0:512], EB[k % 3][:, hh, c0:512], ALU.mult),
                              reads=["WT%d_%d" % (hh, p), "E%d_%d" % (hh, k % 3)], writes=["WT%d_%d" % (hh, p)])

                def emit_ACC(k):
                    kb, j, c0 = geom(k)
                    p = k % 2
                    for hh in range(2):
                        if c0 > 0:
                            S.pool(lambda e, hh=hh: e.memset(ACB[1 - p][:, hh, 0:c0], 0.0), writes=["acb%d_%d" % (hh, 1 - p)])
                        S.dve(lambda e, hh=hh: e.tensor_tensor(ACB[1 - p][:, hh, c0:512], ACB[p][:, hh, c0:512], SPB[p][:, hh, c0:512], ALU.add),
                              reads=["acb%d_%d" % (hh, p), "SP%d_%d" % (hh, p)], writes=["acb%d_%d" % (hh, 1 - p)])

                def emit_PV(k):
                    kb, j, c0 = geom(k)
                    p = k % 2
                    for hh in range(2):
                        hb = hh * 64
                        ob = 6
                        vcol = hp * 128 + hb
                        S.pe(lambda e, ob=ob, hb=hb, kb=kb, vcol=vcol, hh=hh, p=p, c0=c0, k=k, nkb=nkb: e.matmul(
                            banks[ob][hb:hb + 64, c0:512], lhsT=v_sb[:, kb, vcol:vcol + 64], rhs=WTB[p][:, hh, c0:512], start=False, stop=(k == nkb - 1)),
                            reads=["vsb%d" % kb, "WT%d_%d" % (hh, p)], writes=["B%d" % ob])

                S.pool(lambda e: e.memset(ACB[0], 0.0), writes=["acb0_0", "acb1_0"])
                for hh in range(2):
                    hb = hh * 64
                    S.pe(lambda e, hh=hh, hb=hb: e.matmul(banks[6][hb:hb + 64, :], lhsT=ZEROS[:, 0:64], rhs=ZEROS, start=True, stop=False),
                         reads=["c:zeros"], writes=["B6"])
                emit_Z(0)
                emit_Z(1)
                emit_E(0)
                emit_Z(2)
                emit_SP(0)
                emit_E(1)
                for k in range(nkb):
                    emit_L(k)
                    if k >= 1:
                        emit_PV(k - 1)
                    if k + 3 < nkb:
                        emit_Z(k + 3)
                    if k + 1 < nkb:
                        emit_SP(k + 1)
                    if k + 2 < nkb:
                        emit_E(k + 2)
                    emit_W(k)
                    if k + 1 < nkb:
                        emit_ACC(k)
                    emit_M(k)
                    blk[0] += 1
                    if pending and blk[0] % 4 == 0:
                        pending.pop(0)()
                emit_PV(nkb - 1)
                S.dve(lambda e, s=s, hp=hp: e.tensor_copy(OSB[:, hp, s * 512:(s + 1) * 512], banks[6]), reads=["B6"], writes=["OSB"])
            for u in pending:
                u()
        if "osbT" in dbg_d:
            S.barrier(dummy)
            odump = carve(FREE_BASE + 24576, [128, 4 * 2048], F32)
            S.dve(lambda e: e.tensor_copy(odump.rearrange("p (a b) -> p a b", a=4), OSB), reads=["OSB"], writes=["odump"])
            dump("osbT", odump, ["odump"])
        S.barrier(dummy)


        if upto == "SB":
            S.stopped = True
        ga = Alloc(ACC_BASE, HT_BASE)
        glrT = ga([128, T], F32)
        wup_aug = ga([128, 256], F32)
        qgT = ga([128, T], BF16)
        kgT = ga([128, T], BF16)
        kg_tm = ga([128, NT, 128], BF16)
        vg_tm = ga([128, NT, 256], BF16)
        rT = ga([128, 2, T], BF16)
        eg_all = ga([128, T], F32)
        eng_b = [ga([128, 128], F32) for _ in range(3)]
        ed_b = [ga([128, 128], F32) for _ in range(3)]
        scb = [[ga([128, 128], BF16) for _ in range(2)] for _ in range(2)]
        sq_b = [[ga([128, 128], F32) for _ in range(2)] for _ in range(2)]
        oraw = [[ga([128, 128], F32) for _ in range(2)] for _ in range(2)]
        rs_b = [[ga([128, 128], F32) for _ in range(2)] for _ in range(2)]
        tn_b = [[ga([128, 128], F32) for _ in range(2)] for _ in range(2)]
        S_f = ga([128, 128], F32)
        S_b = [ga([128, 128], BF16) for _ in range(2)]
        normw = ga([128, 4], F32)
        etm = [ga([128, 256], F32) for _ in range(2)]
        gf = Alloc(FREE_BASE + 24576, END)
        sp_tm = gf([128, NT, 256], F32)
        TRI_I = gf([128, 128], F32)
        TRI_A = gf([128, 128], F32)
        M2 = gf([128, 128], F32)
        S.pool(lambda e: e.memset(TRI_I, -1.0 / 16.0), writes=["c:trii"])
        S.pool(lambda e: e.affine_select(out=TRI_I, in_=TRI_I, pattern=[[1, 128]], compare_op=ALU.is_ge, fill=0.0, base=0,
                                         channel_multiplier=-1), reads=["c:trii"], writes=["c:trii"])
        S.pool(lambda e: e.memset(TRI_I[0:64, 64:128], 0.0), reads=["c:trii"], writes=["c:trii"])
        S.pool(lambda e: e.memset(TRI_A, -1.0 / 16.0), writes=["c:tria"])
        S.pool(lambda e: e.affine_select(out=TRI_A, in_=TRI_A, pattern=[[-1, 128]], compare_op=ALU.is_gt, fill=0.0, base=0,
                                         channel_multiplier=1), reads=["c:tria"], writes=["c:tria"])
        S.pool(lambda e: e.memset(TRI_A[64:128, 0:64], 0.0), reads=["c:tria"], writes=["c:tria"])
        S.pool(lambda e: e.memset(M2, 1.0), writes=["c:m2"])
        S.pool(lambda e: e.affine_select(out=M2, in_=M2, pattern=[[1, 128]], compare_op=ALU.is_ge, fill=0.0, base=0,
                                         channel_multiplier=-1), reads=["c:m2"], writes=["c:m2"])
        S.pool(lambda e: e.memset(M2[0:64, 64:128], 0.0), reads=["c:m2"], writes=["c:m2"])
        S.pool(lambda e: e.memset(glrT[0:32, :], 1.0), writes=["glrT"])
        S.dma("sp", lambda e: e.dma_start(out=wup_aug[0:16, :], in_=wup_d), "wup", writes=["wup"])
        S.dma("sp", lambda e: e.dma_start(out=wup_aug[16:17, :], in_=bgate_d), "wup", writes=["wup"])
        S.dma("sp", lambda e: e.dma_start(out=normw, in_=normw_d), "normw", writes=["normw"])
        wglr, wglrk = load_w(3072, 16)
        for g in range(NG):
            proj_fm(wglr, wglrk, g, 6, M=16)
            S.act(lambda e, g=g: e.copy(glrT[0:16, g * 512:(g + 1) * 512], banks[6][0:16, :]), reads=["B6"], writes=["glrT"])
        for tt in range(NT):
            p = tt % 2
            S.pe(lambda e, p=p, tt=tt: e.matmul(banks[p][:, 0:256], lhsT=glrT[0:17, tt * 128:(tt + 1) * 128], rhs=wup_aug[0:17, :],
                                                start=True, stop=True), reads=["glrT", "wup"], writes=["B%d" % p])
            S.act(lambda e, p=p: e.activation(etm[p], banks[p][:, 0:256], AF.Exp, scale=-1.0), reads=["B%d" % p], writes=["etm%d" % p])
            S.act(lambda e, p=p, tt=tt: e.activation(sp_tm[:, tt, :], etm[p], AF.Ln, bias=1.0), reads=["etm%d" % p], writes=["sptm%d" % tt])
        for gp in range(2):
            wqg, wqgk = load_w(1536 + gp * 128, 128)
            wkg, wkgk = load_w(1792 + gp * 128, 128)
            wvg, wvgk = load_w(2048 + gp * 256, 256)
            for g in range(NG):
                tk = [4 * g + j for j in range(4)]
                proj_fm(wqg, wqgk, g, 6)
                S.act(lambda e, g=g: e.activation(qgT[:, g * 512:(g + 1) * 512], banks[6], AF.Copy, scale=0.125),
                      reads=["B6"], writes=["qg%d" % t for t in tk])
                proj_fm(wkg, wkgk, g, 7)
                S.dve(lambda e, g=g: e.tensor_copy(kgT[:, g * 512:(g + 1) * 512], banks[7]), reads=["B7"], writes=["kg%d" % t for t in tk])
            for tt in range(NT):
                proj_tm(wkg, wkgk, tt, 6, 128)
                S.act(lambda e, tt=tt: e.copy(kg_tm[:, tt, :], banks[6][:, 0:128]), reads=["B6"], writes=["kgtm%d" % tt])
                proj_tm(wvg, wvgk, tt, 7, 256)
                S.dve(lambda e, tt=tt: e.tensor_copy(vg_tm[:, tt, :], banks[7][:, 0:256]), reads=["B7"], writes=["vgtm%d" % tt])
            wrg, wrgk = load_w(2560 + gp * 256, 256)
            for hh in range(2):
                for g in range(NG):
                    proj_fm(wrg[:, :, hh * 128:(hh + 1) * 128], wrgk, g, 6 + g % 2)
                    S.act(lambda e, g=g, hh=hh: e.activation(rT[:, hh, g * 512:(g + 1) * 512], banks[6 + g % 2], AF.Silu),
                          reads=["B%d" % (6 + g % 2)], writes=["rT%d_%d" % (hh, g)])
            S.pool(lambda e: e.memset(S_f, 0.0), writes=["S_f"])
            S.pool(lambda e: e.memset(S_b[1], 0.0), writes=["S_b1"])

            def stA(tt):
                p3 = tt % 3
                cs = slice(tt * 128, (tt + 1) * 128)
                spk = "sptm%d" % tt
                spsl = sp_tm[:, tt, gp * 128:(gp + 1) * 128]
                S.pe(lambda e: e.matmul(banks[0][:, 0:128], lhsT=spsl, rhs=TRI_I, start=True, stop=True), reads=[spk, "c:trii"], writes=["B0"])
                S.pe(lambda e: e.matmul(banks[0][:, 128:256], lhsT=TRI_A, rhs=spsl, start=True, stop=True), reads=[spk, "c:tria"], writes=["B0"])
                S.act(lambda e: e.activation(eg_all[:, cs], banks[0][:, 0:128], AF.Exp), reads=["B0"], writes=["eg%d" % tt])
                S.act(lambda e: e.activation(eng_b[p3], banks[0][:, 0:128], AF.Exp, scale=-1.0), reads=["B0"], writes=["eng%d" % p3])
                S.act(lambda e: e.activation(ed_b[p3], banks[0][:, 128:256], AF.Exp), reads=["B0"], writes=["ed%d" % p3])
                S.dve(lambda e: e.tensor_tensor(qgT[:, cs], qgT[:, cs], eg_all[:, cs], ALU.mult), reads=["qg%d" % tt, "eg%d" % tt], writes=["qg%d" % tt])
                S.pool(lambda e: e.tensor_tensor(kgT[:, cs], kgT[:, cs], eng_b[p3], ALU.mult), reads=["kg%d" % tt, "eng%d" % p3], writes=["kg%d" % tt])
                S.dve(lambda e: e.tensor_tensor(kg_tm[:, tt, :], kg_tm[:, tt, :], ed_b[p3], ALU.mult), reads=["kgtm%d" % tt, "ed%d" % p3], writes=["kgtm%d" % tt])

            def stB_sc(tt):
                p = tt % 2
                cs = slice(tt * 128, (tt + 1) * 128)
                for hh in range(2):
                    hb = hh * 64
                    sb_ = 2 - hh
                    S.pe(lambda e, hb=hb, sb_=sb_: e.matmul(banks[sb_][:, 0:128], lhsT=kgT[hb:hb + 64, cs], rhs=qgT[hb:hb + 64, cs], start=True, stop=True),
                         reads=["kg%d" % tt, "qg%d" % tt], writes=["B%d" % sb_])
                for hh in range(2):
                    sb_ = 2 - hh
                    S.dve(lambda e, hh=hh, sb_=sb_: e.tensor_tensor(scb[p][hh], banks[sb_][:, 0:128], M2, ALU.mult), reads=["B%d" % sb_, "c:m2"], writes=["scb%d_%d" % (p, hh)])

            def stB_intra(tt):
                p = tt % 2
                for hh in range(2):
                    ob = 3 + 2 * p + hh
                    S.pe(lambda e, ob=ob, hh=hh: e.matmul(banks[ob][:, 0:128], lhsT=vg_tm[:, tt, hh * 128:(hh + 1) * 128], rhs=scb[p][hh], start=True, stop=False),
                         reads=["vgtm%d" % tt, "scb%d_%d" % (p, hh)], writes=["B%d" % ob])

            def stC_inter(tt, ch):
                p = tt % 2
                sprev = 1 - ch
                for hh in range(2):
                    hb = hh * 64
                    ob = 3 + 2 * p + hh
                    S.pe(lambda e, ob=ob, hb=hb: e.matmul(banks[ob][:, ch * 64:ch * 64 + 64], lhsT=S_b[sprev][hb:hb + 64, :],
                                                          rhs=qgT[hb:hb + 64, tt * 128 + ch * 64:tt * 128 + ch * 64 + 64], start=False, stop=(ch == 1)),
                         reads=["S_b%d" % sprev, "qg%d" % tt], writes=["B%d" % ob])

            def stC_U(tt, ch):
                rows = slice(ch * 64, ch * 64 + 64)
                for hh in range(2):
                    hb = hh * 64
                    S.pe(lambda e, hb=hb, hh=hh: e.matmul(banks[7][hb:hb + 64, 0:128], lhsT=kg_tm[rows, tt, hb:hb + 64],
                                                          rhs=vg_tm[rows, tt, hh * 128:(hh + 1) * 128], start=True, stop=True),
                         reads=["kgtm%d" % tt, "vgtm%d" % tt], writes=["B7"])
                col = tt * 128 + ch * 64 + 63
                S.dve(lambda e: e.scalar_tensor_tensor(S_f, S_f, eg_all[:, col:col + 1], banks[7][:, 0:128], ALU.mult, ALU.add),
                      reads=["S_f", "eg%d" % tt, "B7"], writes=["S_f"])
                S.act(lambda e: e.copy(S_b[ch], S_f), reads=["S_f"], writes=["S_b%d" % ch])

            def stP_pre(tt):
                p = tt % 2
                for hh in range(2):
                    ob = 3 + 2 * p + hh
                    S.dve(lambda e, hh=hh, ob=ob: e.tensor_copy(oraw[p][hh], banks[ob][:, 0:128]), reads=["B%d" % ob], writes=["oraw%d_%d" % (p, hh)])
                for hh in range(2):
                    S.act(lambda e, hh=hh: e.activation(sq_b[p][hh], oraw[p][hh], AF.Square), reads=["oraw%d_%d" % (p, hh)], writes=["sq%d_%d" % (p, hh)])

            def stP_ms(tt):
                p = tt % 2
                for hh in range(2):
                    S.pe(lambda e, hh=hh: e.matmul(banks[0][:, 256 + hh * 128:256 + (hh + 1) * 128], lhsT=onesdiv_f, rhs=sq_b[p][hh], start=True, stop=True),
                         reads=["sq%d_%d" % (p, hh), "c:onesdiv"], writes=["B0"])

            def stP_fin(tt):
                p = tt % 2
                cs = slice(tt * 128, (tt + 1) * 128)
                for hh in range(2):
                    S.act(lambda e, hh=hh: e.activation(rs_b[p][hh], banks[0][:, 256 + hh * 128:256 + (hh + 1) * 128], AF.Ln, bias=EPS), reads=["B0"], writes=["rs%d_%d" % (p, hh)])
                for hh in range(2):
                    S.act(lambda e, hh=hh: e.activation(rs_b[p][hh], rs_b[p][hh], AF.Exp, scale=-0.5), reads=["rs%d_%d" % (p, hh)], writes=["rs%d_%d" % (p, hh)])
                for hh in range(2):
                    S.dve(lambda e, hh=hh: e.tensor_tensor(tn_b[p][hh], oraw[p][hh], rs_b[p][hh], ALU.mult),
                          reads=["oraw%d_%d" % (p, hh), "rs%d_%d" % (p, hh)], writes=["tn%d_%d" % (p, hh)])
                for hh in range(2):
                    hd = gp * 2 + hh
                    S.dve(lambda e, hh=hh, hd=hd: e.scalar_tensor_tensor(OG[:, hd, cs], tn_b[p][hh], normw[:, hd:hd + 1], rT[:, hh, cs], ALU.mult, ALU.mult),
                          reads=["tn%d_%d" % (p, hh), "normw", "rT%d_%d" % (hh, tt // 4)], writes=["OG"])

            stA(0)
            stA(1)
            stB_sc(0)
            stB_intra(0)
            for tt in range(NT):
                nx = tt + 1 < NT
                if tt + 2 < NT:
                    stA(tt + 2)
                stC_inter(tt, 0)
                stC_U(tt, 0)
                if nx:
                    stB_sc(tt + 1)
                if tt >= 1:
                    stP_ms(tt - 1)
                if nx:
                    stB_intra(tt + 1)
                stC_inter(tt, 1)
                stC_U(tt, 1)
                stP_pre(tt)
                if tt >= 1:
                    stP_fin(tt - 1)
            stP_ms(NT - 1)
            stP_fin(NT - 1)
        if "ogT" in dbg_d:
            S.barrier(dummy)
            gdump = carve(FREE_BASE + 24576, [128, 4 * 2048], F32)
            S.dve(lambda e: e.tensor_copy(gdump.rearrange("p (a b) -> p a b", a=4), OG), reads=["OG"], writes=["gdump"])
            dump("ogT", gdump, ["gdump"])
        S.barrier(dummy)


        if upto == "GLA":
            S.stopped = True
        GATES = carve(END - 4096, [128, NT, E], F32)
        c1 = Alloc(FREE_BASE, END - 4096)
        yT = c1([128, NG, 8 * 512], BF16)
        wsb_c = [c1([128, 4, 128], BF16) for _ in range(2)]
        wgl_c = [c1([128, 4, 128], BF16) for _ in range(2)]
        wm1_c = [c1([128, 8, 128], BF16) for _ in range(2)]
        wm2_c = [c1([128, 8, 128], BF16) for _ in range(2)]
        s1b = [c1([128, 512], BF16) for _ in range(2)]
        s2b = [c1([128, 512], BF16) for _ in range(2)]
        t1b = [c1([128, 512], BF16) for _ in range(2)]
        t2b = [c1([128, 512], BF16) for _ in range(2)]
        wsb_v = wsb_d.rearrange("(kc p) n -> p kc n", p=128)
        wgl_v = wgla_d.rearrange("(kc p) n -> p kc n", p=128)
        def c1_load(oc):
            q = oc % 2
            ocs = slice(oc * 128, (oc + 1) * 128)
            S.dma("pool", lambda e: e.dma_start(out=wsb_c[q], in_=wsb_v[:, :, ocs]), "wsbc%d" % q, writes=["wsbc%d" % q])
            S.dma("pool", lambda e: e.dma_start(out=wgl_c[q], in_=wgl_v[:, :, ocs]), "wglc%d" % q, writes=["wglc%d" % q])
            S.dma("pool", lambda e: e.dma_start(out=wm1_c[q], in_=win_v[:, :, 3088 + oc * 128:3088 + (oc + 1) * 128]),
                  "wm1c%d" % q, writes=["wm1c%d" % q])
            S.dma("pool", lambda e: e.dma_start(out=wm2_c[q], in_=win_v[:, :, 4112 + oc * 128:4112 + (oc + 1) * 128]),
                  "wm2c%d" % q, writes=["wm2c%d" % q])

        c1_load(0)
        for oc in range(8):
            q = oc % 2
            if oc + 1 < 8:
                c1_load(oc + 1)
            for g in range(NG):
                r = g % 2
                bA, bB, bM1, bM2 = 4 * r, 4 * r + 1, 4 * r + 2, 4 * r + 3
                gs = slice(g * 512, (g + 1) * 512)
                for kc in range(4):
                    S.pe(lambda e, bA=bA, q=q, kc=kc, gs=gs: e.matmul(banks[bA], lhsT=wsb_c[q][:, kc, :], rhs=OSB[:, kc, gs], start=(kc == 0), stop=(kc == 3)),
                         reads=["wsbc%d" % q, "OSB"], writes=["B%d" % bA])
                for kc in range(4):
                    S.pe(lambda e, bB=bB, q=q, kc=kc, gs=gs: e.matmul(banks[bB], lhsT=wgl_c[q][:, kc, :], rhs=OG[:, kc, gs], start=(kc == 0), stop=(kc == 3)),
                         reads=["wglc%d" % q, "OG"], writes=["B%d" % bB])
                for kc in range(8):
                    S.pe(lambda e, bM1=bM1, q=q, kc=kc, gs=gs: e.matmul(banks[bM1], lhsT=wm1_c[q][:, kc, :], rhs=HT[:, kc, gs], start=(kc == 0), stop=(kc == 7)),
                         reads=["wm1c%d" % q, "HT%d_%d" % (g, kc)], writes=["B%d" % bM1])
                for kc in range(8):
                    S.pe(lambda e, bM2=bM2, q=q, kc=kc, gs=gs: e.matmul(banks[bM2], lhsT=wm2_c[q][:, kc, :], rhs=HT[:, kc, gs], start=(kc == 0), stop=(kc == 7)),
                         reads=["wm2c%d" % q, "HT%d_%d" % (g, kc)], writes=["B%d" % bM2])
                S.act(lambda e, r=r, bM1=bM1: e.activation(s1b[r], banks[bM1], AF.Sigmoid), reads=["B%d" % bM1], writes=["s1b%d" % r])
                S.act(lambda e, r=r, bM2=bM2: e.activation(s2b[r], banks[bM2], AF.Sigmoid), reads=["B%d" % bM2], writes=["s2b%d" % r])
                S.dve(lambda e, r=r, bA=bA: e.tensor_tensor(t1b[r], banks[bA], s1b[r], ALU.mult), reads=["B%d" % bA, "s1b%d" % r], writes=["t1b%d" % r])
                S.dve(lambda e, r=r, bB=bB: e.tensor_tensor(t2b[r], banks[bB], s2b[r], ALU.mult), reads=["B%d" % bB, "s2b%d" % r], writes=["t2b%d" % r])
                S.pool(lambda e, r=r, g=g, oc=oc: e.tensor_tensor(yT[:, g, oc * 512:(oc + 1) * 512], t1b[r], t2b[r], ALU.add),
                       reads=["t1b%d" % r, "t2b%d" % r], writes=["yT%d" % g])
        S.barrier(dummy)

        if upto == "C1":
            S.stopped = True
        c2 = Alloc(OSB_BASE, FREE_BASE)
        wout_s = c2([128, 8, D], BF16)
        xt2 = [c2([128, D], F32) for _ in range(2)]
        xnf = [c2([128, D], F32) for _ in range(2)]
        c2b = Alloc(FREE_BASE + 32768, END - 4096)
        h2f = [c2b([128, 8, 128], F32) for _ in range(2)]
        wr_s = c2b([128, 8, E], F32)
        LOG_all = c2b([128, NT, E], F32)
        rb_bc = c2b([128, E], F32)
        stt2 = [c2b([128, 2, 6], F32) for _ in range(2)]
        mvt2 = [c2b([128, 2], F32) for _ in range(2)]
        rst2 = [c2b([128, 1], F32) for _ in range(2)]
        nmr2 = [c2b([128, 1], F32) for _ in range(2)]
        wout_v = wout_d.rearrange("(kc p) n -> p kc n", p=128)
        for hf in range(2):
            S.dma("pool", lambda e, hf=hf: e.dma_start(out=wout_s[:, :, hf * 512:(hf + 1) * 512], in_=wout_v[:, :, hf * 512:(hf + 1) * 512]),
                  "wout%d" % hf, writes=["wout%d" % hf])
        S.dma("sp", lambda e: e.dma_start(out=wr_s, in_=wr_d.rearrange("(kc p) n -> p kc n", p=128)), "wr", writes=["wr"])
        S.dma("sp", lambda e: e.dma_start(out=rb_bc, in_=rb_d.partition_broadcast(128)), "rb", writes=["rb"])
        S.dma("sp", lambda e: e.dma_start(out=lnbc[:, 0, :], in_=ln1g_d.partition_broadcast(128)), "lnbc", writes=["lnbc"])
        S.dma("sp", lambda e: e.dma_start(out=lnbc[:, 1, :], in_=ln1b_d.partition_broadcast(128)), "lnbc", writes=["lnbc"])
        for hf in range(2):
            for kc in range(8):
                S.dve(lambda e, kc=kc, hf=hf: e.tensor_tensor(wout_s[:, kc, hf * 512:(hf + 1) * 512], wout_s[:, kc, hf * 512:(hf + 1) * 512],
                                                              gate1_bc[:, hf * 512:(hf + 1) * 512], ALU.mult),
                      reads=["wout%d" % hf, "gatebc0"], writes=["wout%d" % hf])

        def ln_stat(src, i, rkey, eps):
            st_, mv_, rs_ = stt2[i], mvt2[i], rst2[i]
            S.dve(lambda e: e.bn_stats(st_[:, 0, :], src[:, 0:512]), reads=[rkey], writes=["stt%d" % i])
            S.dve(lambda e: e.bn_stats(st_[:, 1, :], src[:, 512:1024]), reads=[rkey], writes=["stt%d" % i])
            S.dve(lambda e: e.bn_aggr(mv_, st_), reads=["stt%d" % i], writes=["mvt%d" % i])
            S.act(lambda e: e.activation(rs_, mv_[:, 1:2], AF.Ln, bias=eps), reads=["mvt%d" % i], writes=["rst%d" % i])
            S.act(lambda e: e.activation(rs_, rs_, AF.Exp, scale=-0.5), reads=["rst%d" % i], writes=["rst%d" % i])

        def ln_nmr(i):
            S.dve(lambda e: e.tensor_scalar(nmr2[i], mvt2[i][:, 0:1], rst2[i], -1.0, ALU.mult, ALU.mult),
                  reads=["mvt%d" % i, "rst%d" % i], writes=["nmr%d" % i])

        def X1(tt):
            g, jt = tt // 4, tt % 4
            i = tt % 2
            S.dma("sp", lambda e: e.dma_start(out=xt2[i], in_=x_d[tt * 128:(tt + 1) * 128, :]), "xt2_%d" % i, writes=["xt2_%d" % i])
            for hf in range(2):
                bk = 2 * i + hf
                for kc in range(8):
                    S.pe(lambda e, bk=bk, hf=hf, kc=kc: e.matmul(banks[bk], lhsT=yT[:, g, kc * 512 + jt * 128:kc * 512 + (jt + 1) * 128],
                                                                rhs=wout_s[:, kc, hf * 512:(hf + 1) * 512], start=(kc == 0), stop=(kc == 7)),
                         reads=["yT%d" % g, "wout%d" % hf], writes=["B%d" % bk])
                S.dve(lambda e, bk=bk, hf=hf: e.scalar_tensor_tensor(xt2[i][:, hf * 512:(hf + 1) * 512], xt2[i][:, hf * 512:(hf + 1) * 512], ALPHA,
                                                                    banks[bk], ALU.mult, ALU.add),
                      reads=["xt2_%d" % i, "B%d" % bk], writes=["xt2_%d" % i])
            ln_stat(xt2[i], i, "xt2_%d" % i, EPS)

        def Y1a(tt):
            i = tt % 2
            ln_nmr(i)
            S.act(lambda e: e.activation(xnf[i], xt2[i], AF.Identity, bias=nmr2[i], scale=rst2[i]),
                  reads=["xt2_%d" % i, "rst%d" % i, "nmr%d" % i], writes=["xnf%d" % i])

        def Y1b(tt):
            i = tt % 2
            S.pool(lambda e: e.tensor_tensor(xnf[i], xnf[i], lnbc[:, 0, :], ALU.mult), reads=["xnf%d" % i, "lnbc"], writes=["xnf%d" % i])
            S.pool(lambda e: e.tensor_tensor(ACC[:, tt, :], xnf[i], lnbc[:, 1, :], ALU.add), reads=["xnf%d" % i, "lnbc"], writes=["acc%d" % tt])

        X1(0)
        for tt in range(NT):
            Y1a(tt)
            if tt + 1 < NT:
                X1(tt + 1)
            Y1b(tt)

        wgu0_pre = carve(OSB_BASE, [128, 8, 512], BF16)
        wd0_pre = carve(OSB_BASE + 16384, [128, 2, D], BF16)
        sgu_v = wsgu_d.rearrange("(kc p) f -> p kc f", p=128)
        sdn_v = wsd_d.rearrange("(j p) o -> p j o", p=128)
        for hf in range(2):
            S.dma("pool", lambda e, hf=hf: e.dma_start(out=wgu0_pre[:, hf * 4:(hf + 1) * 4, :], in_=sgu_v[:, hf * 4:(hf + 1) * 4, :]),
                  "wgu0", writes=["wgu0", "wout0", "wout1"])
        S.dma("pool", lambda e: e.dma_start(out=wd0_pre, in_=sdn_v), "wd0", writes=["wd0", "xt2_0"])
        for j in range(2):
            S.pool(lambda e, j=j: e.tensor_tensor(wd0_pre[:, j, :], wd0_pre[:, j, :], gate2_bc, ALU.mult),
                   reads=["wd0", "gatebc1"], writes=["wd0"])

        def Y2a(tt):
            i = tt % 2
            ln_nmr(i)
            S.act(lambda e: e.activation(xnf[i], ACC[:, tt, :], AF.Identity, bias=nmr2[i], scale=rst2[i]),
                  reads=["acc%d" % tt, "rst%d" % i, "nmr%d" % i], writes=["xnf%d" % i])
            for kc in range(8):
                bi = 4 + 2 * i + kc // 4
                S.pe(lambda e, bi=bi, kc=kc: e.transpose(banks[bi][:, (kc % 4) * 128:(kc % 4 + 1) * 128], xnf[i][:, kc * 128:(kc + 1) * 128], ident_f),
                     reads=["xnf%d" % i, "c:identf"], writes=["B%d" % bi])

        def Y2b(tt):
            i = tt % 2
            for kc in range(8):
                bi = 4 + 2 * i + kc // 4
                src = banks[bi][:, (kc % 4) * 128:(kc % 4 + 1) * 128]
                if kc < 4:
                    S.act(lambda e, src=src, kc=kc: e.activation(h2f[i][:, kc, :], src, AF.Identity, bias=modcol[:, 24 + kc:25 + kc], scale=modcol[:, 16 + kc:17 + kc]),
                          reads=["B%d" % bi, "modcol"], writes=["h2f%d_%d" % (i, kc)])
                else:
                    S.dve(lambda e, src=src, kc=kc: e.tensor_scalar(h2f[i][:, kc, :], src, modcol[:, 16 + kc:17 + kc], modcol[:, 24 + kc:25 + kc], ALU.mult, ALU.add),
                          reads=["B%d" % bi, "modcol"], writes=["h2f%d_%d" % (i, kc)])

        def Z2a(tt):
            i = tt % 2
            for kc in range(8):
                S.pe(lambda e, kc=kc: e.matmul(banks[i][:, 0:E], lhsT=h2f[i][:, kc, :], rhs=wr_s[:, kc, :], start=(kc == 0), stop=(kc == 7)),
                     reads=["h2f%d_%d" % (i, kc), "wr"], writes=["B%d" % i])

        def Z2b(tt):
            i = tt % 2
            h2k = ["h2f%d_%d" % (i, kc) for kc in range(8)]
            S.pool(lambda e: e.tensor_copy(HT[:, :, tt * 128:(tt + 1) * 128], h2f[i]), reads=h2k, writes=["h2T%d" % tt])
            S.dve(lambda e: e.tensor_copy(LOG_all[:, tt, :], banks[i][:, 0:E]), reads=["B%d" % i], writes=["logits"])

        ln_stat(ACC[:, 0, :], 0, "acc0", EPS)
        for tt in range(NT):
            if tt >= 1:
                Z2a(tt - 1)
            Y2a(tt)
            if tt + 1 < NT:
                ln_stat(ACC[:, tt + 1, :], (tt + 1) % 2, "acc%d" % (tt + 1), EPS)
            if tt >= 1:
                Z2b(tt - 1)
            Y2b(tt)
        Z2a(NT - 1)
        Z2b(NT - 1)
        if "h2T" in dbg_d:
            S.barrier(dummy)
            h2dump = carve(OSB_BASE, [128, 8 * 2048], F32)
            S.dve(lambda e: e.tensor_copy(h2dump.rearrange("p (a b) -> p a b", a=8), HT), reads=[], writes=["h2dump"])
            dump("h2T", h2dump, ["h2dump"])
        S.barrier(dummy)
        def emit_pass3():
            c3 = Alloc(FREE_BASE + 8192, END - 4096)
            sc_all = c3([128, NT, E], F32)
            sel_all = c3([128, NT, E], F32)
            selm_all = c3([128, NT * 8, 8], F32)
            m8a = c3([128, NT * 8, 8], F32)
            gs_all = c3([128, NT * 8], F32)
            gm8_all = c3([128, NT, 8], F32)
            gmask_all = c3([128, NT * 8], F32)
            gneg_all = c3([128, NT * 8], F32)
            e8_all = c3([128, NT, 8], F32)
            cho_all = c3([128, NT, E], F32)
            wsum_all = c3([128, NT], F32)
            wrec_all = c3([128, NT], F32)
            S.act(lambda e: e.activation(sc_all, LOG_all, AF.Sigmoid), reads=["logits"], writes=["sc_all"])
            for tt in range(NT):
                S.dve(lambda e, tt=tt: e.tensor_tensor(sel_all[:, tt, :], sc_all[:, tt, :], rb_bc, ALU.add), reads=["sc_all", "rb"], writes=["sel%d" % tt])
            sel_g = sel_all.rearrange("p a (g k) -> p (a g) k", k=8)
            for tt in range(NT):
                for gi in range(8):
                    S.dve(lambda e, tt=tt, gi=gi: e.max(out=m8a[:, tt * 8 + gi, :], in_=sel_g[:, tt * 8 + gi, :]), reads=["sel%d" % tt], writes=["m8a%d_%d" % (tt, gi)])
            S.dve(lambda e: e.tensor_tensor(gs_all, m8a[:, :, 0], m8a[:, :, 1], ALU.add),
                  reads=["m8a%d_%d" % (t, gi) for t in range(NT) for gi in range(8)], writes=["gs_all"])
            for tt in range(NT):
                S.dve(lambda e, tt=tt: e.max(out=gm8_all[:, tt, :], in_=gs_all[:, tt * 8:(tt + 1) * 8]), reads=["gs_all"], writes=["gm8_%d" % tt])
            for tt in range(NT):
                S.dve(lambda e, tt=tt: e.tensor_scalar(gmask_all[:, tt * 8:(tt + 1) * 8], gs_all[:, tt * 8:(tt + 1) * 8], gm8_all[:, tt, 3:4], None, ALU.is_ge),
                      reads=["gs_all", "gm8_%d" % tt], writes=["gmask%d" % tt])
            gmk = ["gmask%d" % t for t in range(NT)]
            S.dve(lambda e: e.tensor_scalar(gneg_all, gmask_all, 1e30, -1e30, ALU.mult, ALU.add), reads=gmk, writes=["gneg"])
            S.dve(lambda e: e.tensor_tensor(selm_all, sel_g, gmask_all.unsqueeze(2).broadcast_to([128, NT * 8, 8]), ALU.mult),
                  reads=gmk + ["sel%d" % t for t in range(NT)], writes=["selm"])
            S.dve(lambda e: e.tensor_tensor(selm_all, selm_all, gneg_all.unsqueeze(2).broadcast_to([128, NT * 8, 8]), ALU.add),
                  reads=["selm", "gneg"], writes=["selm"])
            selm_t = selm_all.rearrange("p (a g) k -> p a (g k)", g=8)
            for tt in range(NT):
                S.dve(lambda e, tt=tt: e.max(out=e8_all[:, tt, :], in_=selm_t[:, tt, :]), reads=["selm"], writes=["e8_%d" % tt])
            for tt in range(NT):
                S.dve(lambda e, tt=tt: e.tensor_scalar(cho_all[:, tt, :], selm_t[:, tt, :], e8_all[:, tt, 7:8], None, ALU.is_ge),
                      reads=["selm", "e8_%d" % tt], writes=["cho%d" % tt])
            chk = ["cho%d" % t for t in range(NT)]
            S.dve(lambda e: e.tensor_tensor(cho_all, cho_all, sc_all, ALU.mult), reads=chk + ["sc_all"], writes=chk)
            S.dve(lambda e: e.reduce_sum(wsum_all, cho_all, AX.X), reads=chk, writes=["wsum"])
            S.dve(lambda e: e.reciprocal(wrec_all, wsum_all), reads=["wsum"], writes=["wrec"])
            for tt in range(NT):
                S.dve(lambda e, tt=tt: e.tensor_scalar(GATES[:, tt, :], cho_all[:, tt, :], wrec_all[:, tt:tt + 1], 2.5, ALU.mult, ALU.mult),
                      reads=["cho%d" % tt, "wrec"], writes=["gates%d" % tt])
            if "gates" in dbg_d:
                dump("gates", GATES.rearrange("p a b -> p (a b)"), ["gates%d" % t for t in range(NT)])

        if "x1" in dbg_d:
            dump("x1", ACC.rearrange("p a b -> p (a b)"), ["acc%d" % t for t in range(NT)])
        if upto == "C2":
            S.stopped = True
        da = Alloc(OSB_BASE, END - 4096)
        wgu_s = [da([128, 8, 512], BF16) for _ in range(2)]
        wd_s = [da([128, 2, D], BF16) for _ in range(2)]
        sgb = [da([128, 512], BF16) for _ in range(2)]
        actT = [da([128, 2, 512], BF16) for _ in range(2)]
        otile = [da([128, D], F32) for _ in range(2)]
        stt3 = [da([128, 2, 6], F32) for _ in range(2)]
        mvt3 = [da([128, 2], F32) for _ in range(2)]
        rst3 = [da([128, 1], F32) for _ in range(2)]
        S.dma("sp", lambda e: e.dma_start(out=lnbc[:, 0, :], in_=ln2g_d.partition_broadcast(128)), "lnbc", writes=["lnbc"])
        S.dma("sp", lambda e: e.dma_start(out=lnbc[:, 1, :], in_=ln2b_d.partition_broadcast(128)), "lnbc", writes=["lnbc"])
        nmr3 = [da([128, 1], F32) for _ in range(2)]

        def emit_final(tt):
            i = tt % 2
            ak = ["acc%d_0" % tt, "acc%d_1" % tt]
            S.dve(lambda e: e.bn_stats(stt3[i][:, 0, :], ACC[:, tt, 0:512]), reads=ak, writes=["stt%d" % i])
            S.dve(lambda e: e.bn_stats(stt3[i][:, 1, :], ACC[:, tt, 512:1024]), reads=ak, writes=["stt%d" % i])
            S.dve(lambda e: e.bn_aggr(mvt3[i], stt3[i]), reads=["stt%d" % i], writes=["mvt%d" % i])
            S.act(lambda e: e.activation(rst3[i], mvt3[i][:, 1:2], AF.Ln, bias=EPS / (ALPHA * ALPHA)), reads=["mvt%d" % i], writes=["rst%d" % i])
            S.act(lambda e: e.activation(rst3[i], rst3[i], AF.Exp, scale=-0.5), reads=["rst%d" % i], writes=["rst%d" % i])
            S.dve(lambda e: e.tensor_scalar(nmr3[i], mvt3[i][:, 0:1], rst3[i], -1.0, ALU.mult, ALU.mult), reads=["mvt%d" % i, "rst%d" % i], writes=["nmr%d" % i])
            S.act(lambda e: e.activation(otile[i], ACC[:, tt, :], AF.Identity, bias=nmr3[i], scale=rst3[i]),
                  reads=ak + ["rst%d" % i, "nmr%d" % i], writes=["otile%d" % i])
            S.dve(lambda e: e.tensor_tensor(otile[i], otile[i], lnbc[:, 0, :], ALU.mult), reads=["otile%d" % i, "lnbc"], writes=["otile%d" % i])
            S.pool(lambda e: e.tensor_tensor(otile[i], otile[i], lnbc[:, 1, :], ALU.add), reads=["otile%d" % i, "lnbc"], writes=["otile%d" % i])
            S.dma("sp", lambda e: e.dma_start(out=out_d[tt * 128:(tt + 1) * 128, :], in_=otile[i]), "out%d" % i, reads=["otile%d" % i])

        order = [E] + list(range(E))

        def moe_load(pos):
            ex = order[pos]
            q = pos % 2
            if ex < E:
                gu_v = wgu_d[ex].rearrange("(kc p) f -> p kc f", p=128)
                dn_v = wd_d[ex].rearrange("(j p) o -> p j o", p=128)
            else:
                gu_v = wsgu_d.rearrange("(kc p) f -> p kc f", p=128)
                dn_v = wsd_d.rearrange("(j p) o -> p j o", p=128)
            for hf in range(2):
                S.dma("pool", lambda e, hf=hf: e.dma_start(out=wgu_s[q][:, hf * 4:(hf + 1) * 4, :], in_=gu_v[:, hf * 4:(hf + 1) * 4, :]),
                      "wgu%d" % q, writes=["wgu%d" % q])
            S.dma("pool", lambda e: e.dma_start(out=wd_s[q], in_=dn_v), "wd%d" % q, writes=["wd%d" % q])
            for j in range(2):
                S.pool(lambda e, j=j: e.tensor_tensor(wd_s[q][:, j, :], wd_s[q][:, j, :], gate2_bc, ALU.mult),
                       reads=["wd%d" % q, "gatebc1"], writes=["wd%d" % q])

        def moe_g_or_u(idx, j, which):
            pos, g = idx // NG, idx % NG
            q = pos % 2
            gs = slice(g * 512, (g + 1) * 512)
            hk = ["h2T%d" % t for t in range(4 * g, 4 * g + 4)]
            bk = j if which == 0 else 2 + j
            c0 = j * 128 if which == 0 else 256 + j * 128
            for kc in range(8):
                S.pe(lambda e, kc=kc: e.matmul(banks[bk], lhsT=wgu_s[q][:, kc, c0:c0 + 128], rhs=HT[:, kc, gs],
                                               start=(kc == 0), stop=(kc == 7)), reads=["wgu%d" % q] + hk, writes=["B%d" % bk])

        def moe_act(idx, j):
            a = idx % 2
            S.act(lambda e: e.activation(sgb[j], banks[j], AF.Silu), reads=["B%d" % j], writes=["sgb%d" % j])
            S.dve(lambda e: e.tensor_tensor(actT[a][:, j, :], banks[2 + j], sgb[j], ALU.mult),
                  reads=["B%d" % (2 + j), "sgb%d" % j], writes=["actT%d_%d" % (a, j)])

        def moe_down_tile(idx, jt):
            pos, g = idx // NG, idx % NG
            ex = order[pos]
            q = pos % 2
            a = idx % 2
            tt = 4 * g + jt
            for hf in range(2):
                bD = 4 + (jt * 2 + hf) % 4
                for j in range(2):
                    S.pe(lambda e, bD=bD, j=j, hf=hf: e.matmul(banks[bD], lhsT=actT[a][:, j, jt * 128:(jt + 1) * 128],
                                                               rhs=wd_s[q][:, j, hf * 512:(hf + 1) * 512], start=(j == 0), stop=(j == 1)),
                         reads=["actT%d_%d" % (a, j), "wd%d" % q], writes=["B%d" % bD])
                accs = ACC[:, tt, hf * 512:(hf + 1) * 512]
                sca = GATES[:, tt, ex:ex + 1] if ex < E else 1.0
                S.dve(lambda e, bD=bD, accs=accs, sca=sca: e.scalar_tensor_tensor(accs, banks[bD], sca, accs, ALU.mult, ALU.add),
                      reads=["B%d" % bD, "acc%d_%d" % (tt, hf), "gates%d" % tt], writes=["acc%d_%d" % (tt, hf)])
            if pos == E and jt == 3:
                for t4 in range(4):
                    emit_final(4 * g + t4)

        nidx = (E + 1) * NG
        for idx in range(nidx + 1):
            pos, g = idx // NG, idx % NG
            live = idx < nidx
            prev = idx - 1 if idx >= 1 else None
            if live and g == 0:
                if pos == 1:
                    emit_pass3()
                if pos >= 1:
                    moe_load(pos)
            if live:
                moe_g_or_u(idx, 0, 0)
            if prev is not None:
                moe_down_tile(prev, 0)
            if live:
                moe_g_or_u(idx, 0, 1)
                moe_act(idx, 0)
            if prev is not None:
                moe_down_tile(prev, 1)
            if live:
                moe_g_or_u(idx, 1, 0)
            if prev is not None:
                moe_down_tile(prev, 2)
            if live:
                moe_g_or_u(idx, 1, 1)
                moe_act(idx, 1)
            if prev is not None:
                moe_down_tile(prev, 3)
        if "acc" in dbg_d:
            dump("acc", ACC.rearrange("p a b -> p (a b)"), ["acc%d_%d" % (t, h) for t in range(NT) for h in range(2)])
        S.emit(final_keys=["out0", "out1"] + (["dbg"] if dbg_d else []))
    return nc


def _in_maps(inputs):
    f = lambda a: np.ascontiguousarray(np.asarray(a, dtype=np.float32))
    shared = {
        "w_ada": f(inputs["w_ada"][0]), "b_ada": f(inputs["b_ada"][0]).reshape(1, -1),
        "w_in": f(inputs["w_in"][0]), "gla_w_gate_up": f(inputs["gla_w_gate_up"][0]),
        "gla_b_gate": f(inputs["gla_b_gate"][0]).reshape(1, -1),
        "gla_norm_w_col": f(np.asarray(inputs["gla_norm_w"][0]).reshape(4, 128).T),
        "w_branch_sb": f(inputs["w_branch_sb"][0]), "w_branch_gla": f(inputs["w_branch_gla"][0]),
        "w_out": f(inputs["w_out"][0]), "ln1_g": f(inputs["ln1_g"][0]).reshape(1, -1), "ln1_b": f(inputs["ln1_b"][0]).reshape(1, -1),
        "w_router": f(inputs["w_router"][0]), "router_bias": f(inputs["router_bias"][0]).reshape(1, -1),
        "w_exp_gate_up": f(inputs["w_exp_gate_up"][0]), "w_exp_down": f(inputs["w_exp_down"][0]),
        "w_shared_gate_up": f(inputs["w_shared_gate_up"][0]), "w_shared_down": f(inputs["w_shared_down"][0]),
        "ln2_g": f(inputs["ln2_g"][0]).reshape(1, -1), "ln2_b": f(inputs["ln2_b"][0]).reshape(1, -1),
    }
    maps = []
    for b in range(8):
        m = dict(shared)
        m["x"] = f(inputs["x"][b])
        m["c_col"] = f(np.asarray(inputs["c"][b]).reshape(8, 128).T)
        maps.append(m)
    return maps


def kernel(**inputs):
    nc = build()
    maps = _in_maps(inputs)
    res = run_bass_kernel_spmd(nc, maps, core_ids=list(range(8)))
    return np.stack([np.asarray(r["out"], dtype=np.float32) for r in res.results], axis=0)
```
